# Optimizing a Trainium2 kernel written in Bass

```python
import jax
import jax.numpy as jnp
from jax import lax
import numpy as np

D_MODEL = 1024
BATCH = 8
SEQ = 2048
DEPTH = 1

EPS = 1e-6
HG_HEADS = 8
HG_DK = 128
HG_DV = D_MODEL // HG_HEADS
HG_KW = HG_HEADS * HG_DK
HG_VW = HG_HEADS * HG_DV
HG_CHUNK = 32
CONV_WIDTH = D_MODEL
CONV_K = 31
IN_SIZES = (HG_KW, HG_KW, HG_VW, HG_VW, CONV_WIDTH, CONV_WIDTH, D_MODEL, D_MODEL)
IN_SPLITS = tuple(int(s) for s in np.cumsum(IN_SIZES)[:-1])
N_IN = sum(IN_SIZES)
PEER_HEADS = 8
PEER_NKEYS = 128
PEER_EXPERTS = PEER_NKEYS * PEER_NKEYS
PEER_DQ = 256
PEER_TOPK = 16
PEER_BLOCK = 128

kernel_name = 'hybrid_hgrn2_conformer_peer_adaln'


def rms_norm(x, g):
    xf = x.astype(jnp.float32)
    y = xf * lax.rsqrt(jnp.mean(xf * xf, axis=-1, keepdims=True) + EPS)
    return (y * g.astype(jnp.float32)).astype(x.dtype)


def layer_norm(x, g, b):
    xf = x.astype(jnp.float32)
    mu = jnp.mean(xf, axis=-1, keepdims=True)
    xc = xf - mu
    y = xc * lax.rsqrt(jnp.mean(xc * xc, axis=-1, keepdims=True) + EPS)
    return (y * g.astype(jnp.float32) + b.astype(jnp.float32)).astype(x.dtype)


def hgrn2_chunkwise(q, k, v, logf):
    B, S, H, dk = q.shape
    dv = v.shape[-1]
    n = S // HG_CHUNK

    def to_chunks(t):
        return t.reshape(B, n, HG_CHUNK, H, t.shape[-1]).transpose(1, 0, 3, 2, 4)

    causal = jnp.tril(jnp.ones((HG_CHUNK, HG_CHUNK), dtype=bool))

    def step(state, inp):
        qt, kt, vt, lt = inp
        bcum = jnp.cumsum(lt, axis=2)
        rel = bcum[:, :, :, None, :] - bcum[:, :, None, :, :]
        decay = jnp.exp(jnp.where(causal[:, :, None], rel, -jnp.inf))
        scores = jnp.einsum('bhtk,bhsk,bhtsk->bhts', qt, kt, decay)
        o = jnp.einsum('bhts,bhsv->bhtv', scores, vt)
        o = o + jnp.einsum('bhtk,bhkv->bhtv', qt * jnp.exp(bcum), state)
        blast = bcum[:, :, -1:, :]
        state = (jnp.exp(blast[:, :, 0, :])[..., None] * state
                 + jnp.einsum('bhsk,bhsv->bhkv', kt * jnp.exp(blast - bcum), vt))
        return state, o

    s0 = jnp.zeros((B, H, dk, dv), jnp.float32)
    _, o = lax.scan(step, s0, (to_chunks(q), to_chunks(k), to_chunks(v), to_chunks(logf)))
    return o.transpose(1, 0, 3, 2, 4).reshape(B, S, H, dv)


def causal_depthwise_conv(u, w, b):
    y = lax.conv_general_dilated(
        u, w[:, None, :].astype(u.dtype), window_strides=(1,),
        padding=((CONV_K - 1, 0),), dimension_numbers=('NWC', 'WIO', 'NWC'),
        feature_group_count=u.shape[-1])
    return y + b


def peer_layer(h, w_q, sub_keys, u_tab, v_tab):
    B, S, D = h.shape
    T = B * S
    hf = h.reshape(T, D)
    q = (hf @ w_q).reshape(T, PEER_HEADS, 2, PEER_DQ // 2)
    s = jnp.einsum('thpd,hpnd->thpn', q, sub_keys)
    sc, idx = lax.top_k(s, PEER_TOPK)
    cand = (sc[:, :, 0, :, None] + sc[:, :, 1, None, :]).reshape(T, PEER_HEADS, PEER_TOPK * PEER_TOPK)
    cidx = (idx[:, :, 0, :, None] * PEER_NKEYS + idx[:, :, 1, None, :]).reshape(T, PEER_HEADS, PEER_TOPK * PEER_TOPK)
    top_sc, pos = lax.top_k(cand, PEER_TOPK)
    eidx = jnp.take_along_axis(cidx, pos, axis=-1)
    gate = jax.nn.softmax(top_sc.astype(jnp.float32), axis=-1).astype(h.dtype)
    nb = T // PEER_BLOCK

    def block(args):
        hb, eb, gb = args
        act = jax.nn.gelu(jnp.einsum('thkd,td->thk', u_tab[eb], hb), approximate=False)
        return jnp.einsum('thk,thkd->td', gb * act, v_tab[eb])

    y = lax.map(block, (hf.reshape(nb, PEER_BLOCK, D),
                        eidx.reshape(nb, PEER_BLOCK, PEER_HEADS, PEER_TOPK),
                        gate.reshape(nb, PEER_BLOCK, PEER_HEADS, PEER_TOPK)))
    return y.reshape(B, S, D)


def setup_inputs(seed: int = 0) -> dict:
    key = jax.random.key(seed)
    ks = jax.random.split(key, 24)
    L = DEPTH

    def nrm(k, shape, scale):
        return jax.random.normal(k, shape, jnp.float32) * scale

    return {
        'x': nrm(ks[0], (BATCH, SEQ, D_MODEL), 1.0),
        'c': nrm(ks[1], (BATCH, D_MODEL), 1.0),
        'ada_w': nrm(ks[2], (L, D_MODEL, 6 * D_MODEL), 0.5 * D_MODEL ** -0.5),
        'ada_b': nrm(ks[3], (L, 6 * D_MODEL), 0.02),
        'norm1_g': 1.0 + nrm(ks[4], (L, D_MODEL), 0.02),
        'w_in': nrm(ks[5], (L, D_MODEL, N_IN), D_MODEL ** -0.5),
        'lb_logits': nrm(ks[6], (L + 1, HG_KW), 0.5),
        'hg_norm_g': 1.0 + nrm(ks[7], (L, HG_VW), 0.02),
        'w_a': nrm(ks[8], (L, HG_VW, D_MODEL), HG_VW ** -0.5),
        'conv_w': nrm(ks[9], (L, CONV_K, CONV_WIDTH), CONV_K ** -0.5),
        'conv_b': nrm(ks[10], (L, CONV_WIDTH), 0.02),
        'conv_ln_g': 1.0 + nrm(ks[11], (L, CONV_WIDTH), 0.02),
        'conv_ln_b': nrm(ks[12], (L, CONV_WIDTH), 0.02),
        'w_b': nrm(ks[13], (L, CONV_WIDTH, D_MODEL), CONV_WIDTH ** -0.5),
        'w_out': nrm(ks[14], (L, D_MODEL, D_MODEL), D_MODEL ** -0.5),
        'norm2_g': 1.0 + nrm(ks[15], (L, D_MODEL), 0.02),
        'peer_wq': nrm(ks[16], (L, D_MODEL, PEER_HEADS * PEER_DQ), D_MODEL ** -0.5),
        'peer_keys': nrm(ks[17], (L, PEER_HEADS, 2, PEER_NKEYS, PEER_DQ // 2), (PEER_DQ // 2) ** -0.5),
        'peer_u': nrm(ks[18], (L, PEER_EXPERTS, D_MODEL), D_MODEL ** -0.5),
        'peer_v': nrm(ks[19], (L, PEER_EXPERTS, D_MODEL), PEER_HEADS ** -0.5),
        'final_g': 1.0 + nrm(ks[20], (D_MODEL,), 0.02),
    }


def reference(x, c, ada_w, ada_b, norm1_g, w_in, lb_logits, hg_norm_g, w_a, conv_w, conv_b,
              conv_ln_g, conv_ln_b, w_b, w_out, norm2_g, peer_wq, peer_keys, peer_u, peer_v,
              final_g):
    B, S, D = x.shape
    lower_bounds = jnp.cumsum(jax.nn.softmax(lb_logits.astype(jnp.float32), axis=0), axis=0)

    def heads(t):
        return t.reshape(B, S, HG_HEADS, -1)

    h = x
    for l in range(DEPTH):
        ada = (jax.nn.silu(c) @ ada_w[l] + ada_b[l])[:, None, :]
        sh1, sc1, g1, sh2, sc2, g2 = jnp.split(ada, 6, axis=-1)

        u = rms_norm(h, norm1_g[l]) * (1 + sc1) + sh1
        z = u @ w_in[l]
        zq, zf, zi, zg, zca, zcb, zga, zgb = jnp.split(z, IN_SPLITS, axis=-1)

        lb = lower_bounds[l]
        f = lb + (1.0 - lb) * jax.nn.sigmoid(zf.astype(jnp.float32))
        o = hgrn2_chunkwise(heads(zq.astype(jnp.float32)), heads(1.0 - f),
                            heads(zi.astype(jnp.float32)), heads(jnp.log(f)))
        o = rms_norm(o, hg_norm_g[l].reshape(HG_HEADS, HG_DV)).reshape(B, S, HG_VW).astype(x.dtype)
        y_a = (o * jax.nn.silu(zg)) @ w_a[l]

        glu = zca * jax.nn.sigmoid(zcb)
        cv = causal_depthwise_conv(glu, conv_w[l], conv_b[l])
        cv = jax.nn.silu(layer_norm(cv, conv_ln_g[l], conv_ln_b[l]))
        y_b = cv @ w_b[l]

        merged = jax.nn.sigmoid(zga) * y_a + jax.nn.sigmoid(zgb) * y_b
        h = h + g1 * (merged @ w_out[l])

        u2 = rms_norm(h, norm2_g[l]) * (1 + sc2) + sh2
        h = h + g2 * peer_layer(u2, peer_wq[l], peer_keys[l], peer_u[l], peer_v[l])

    return rms_norm(h, final_g)
```

```python
import numpy as np
from contextlib import ExitStack
import concourse.bass as bass
import concourse.mybir as mybir
from concourse.bass_utils import run_bass_kernel_spmd

F32 = mybir.dt.float32
BF16 = mybir.dt.bfloat16
U32 = mybir.dt.uint32
I32 = mybir.dt.int32
AF = mybir.ActivationFunctionType
ALU = mybir.AluOpType
AX = mybir.AxisListType

S = 2048
D = 1024
NT = 16
EPS = 1e-6
NCORES = 8


class Prog:
    def __init__(self, nc):
        self.nc = nc
        self.stack = ExitStack()
        self.E = dict(pe=nc.tensor, act=nc.scalar, dve=nc.vector, pool=nc.gpsimd, sp=nc.sync)
        self.sem = {k: self.stack.enter_context(nc.semaphore("sem_" + k)) for k in self.E}
        self.cnt = {k: 0 for k in self.E}
        self.known = {k: {} for k in self.E}
        self.NDS = 64
        self.dsem = [self.stack.enter_context(nc.semaphore(f"dsem{i}")) for i in range(self.NDS)]
        self.dcnt = [0] * self.NDS
        self.dnext = 0
        self.dnext_q = {}
        self.res = {}
        self.nins = 0

    def _wait(self, e, key, val):
        if val <= 0:
            return
        if e == 'pe' and key == 'pe':
            return
        if self.known[e].get(key, 0) >= val:
            return
        sem = self.sem[key] if isinstance(key, str) else self.dsem[key[1]]
        self.E[e].wait_ge(sem, val)
        self.known[e][key] = val

    def _deps(self, r, w):
        deps = {}

        def add(k, v):
            if deps.get(k, 0) < v:
                deps[k] = v
        for t in r:
            st = self.res.get(t)
            if st and st[0]:
                add(*st[0])
        for t in w:
            st = self.res.get(t)
            if st:
                if st[0]:
                    add(*st[0])
                for k, v in st[1].items():
                    add(k, v)
        return deps

    def _record(self, me, r, w):
        for t in r:
            st = self.res.setdefault(t, [None, {}])
            if st[1].get(me[0], 0) < me[1]:
                st[1][me[0]] = me[1]
        for t in w:
            self.res[t] = [me, {}]

    def op(self, e, fn, r=(), w=()):
        for k, v in self._deps(r, w).items():
            self._wait(e, k, v)
        ins = fn(self.E[e])
        self.cnt[e] += 1
        ins.then_inc(self.sem[e], 1)
        self._record((e, self.cnt[e]), r, w)
        self.nins += 1

    def dma(self, q, fn, r=(), w=()):
        for k, v in self._deps(r, w).items():
            self._wait(q, k, v)
        half = self.NDS // 2
        base = 0 if q == 'sp' else half
        i = base + self.dnext_q.get(q, 0)
        self.dnext_q[q] = (self.dnext_q.get(q, 0) + 1) % half
        self._wait(q, ('d', i), self.dcnt[i])
        ins = fn(self.E[q])
        self.dcnt[i] += 16
        ins.then_inc(self.dsem[i], 16)
        self._record((('d', i), self.dcnt[i]), r, w)
        self.nins += 1

    def barrier(self, skip_pool_dma=False):
        nd = self.NDS // 2 if skip_pool_dma else self.NDS
        for e in self.E:
            if skip_pool_dma and e == 'pool':
                continue
            for k in self.E:
                if k != e:
                    self._wait(e, k, self.cnt[k])
            for i in range(nd):
                self._wait(e, ('d', i), self.dcnt[i])

    def finish(self, tokens):
        for t in tokens:
            st = self.res.get(t)
            if st and st[0]:
                self._wait('sp', st[0][0], st[0][1])


def build_nc(stage=99, dbg_shape=None):
    nc = bass.Bass("TRN2", target_bir_lowering=False)
    dram = {}

    def din(name, shape):
        dram[name] = nc.dram_tensor(name, list(shape), F32, kind="ExternalInput").ap()
        return dram[name]
    x = din("x", [S, D])
    c = din("c", [D])
    ada_w = din("ada_w", [D, 6 * D])
    ada_b = din("ada_b", [6 * D])
    norm1_g = din("norm1_g", [D])
    w_in = din("w_in", [D, 8 * D])
    lb_logits = din("lb_logits", [2, D])
    hg_norm_g = din("hg_norm_g", [D])
    w_a = din("w_a", [D, D])
    conv_w = din("conv_w", [31, D])
    conv_b = din("conv_b", [D])
    conv_ln_g = din("conv_ln_g", [D])
    conv_ln_b = din("conv_ln_b", [D])
    w_b = din("w_b", [D, D])
    w_out = din("w_out", [D, D])
    norm2_g = din("norm2_g", [D])
    peer_wq = din("peer_wq", [D, 2 * D])
    peer_keys = din("peer_keys", [16, 128, 128])
    peer_u = din("peer_u", [16384, D])
    peer_v = din("peer_v", [16384, D])
    final_g = din("final_g", [D])
    out = nc.dram_tensor("out", [S, D], F32, kind="ExternalOutput").ap()
    tabUV = nc.dram_tensor("tabUV", [16384, 2048], BF16, kind="Internal").ap()
    h1s = nc.dram_tensor("h1s", [S, D], F32, kind="Internal").ap()
    dbg = None
    if dbg_shape is not None:
        dbg = nc.dram_tensor("dbg", list(dbg_shape), F32, kind="ExternalOutput").ap()

    P = Prog(nc)
    ES = ExitStack()

    def sb(name, shape, dt=F32, stack=None):
        return (stack or ES).enter_context(nc.sbuf_tensor(name, list(shape), dt))

    def ps(name, shape, dt=F32):
        return ES.enter_context(nc.psum_tensor(name, list(shape), dt))

    with P.stack, ES:
        zp = [ps("zp0", [128, 1024]), ps("zp1", [128, 1024])]
        gp = [ps(f"gp{i}", [128, 512]) for i in range(4)]
        ZT = [["zp0a", "zp0b"], ["zp1a", "zp1b"]]
        GT = ["gp0", "gp1", "gp2", "gp3"]

        ident = sb("ident", [128, 128], F32)
        identb = sb("identb", [128, 128], BF16)
        ones_b = sb("ones_b", [128, 128], BF16)
        onesD_b = sb("onesD_b", [128, 128], BF16)
        ones_f = sb("ones_f", [128, 128], F32)
        iot_i = sb("iot_i", [128, 128], I32)
        iop_i = sb("iop_i", [128, 128], I32)
        P.op('pool', lambda e: e.iota(iot_i[:], [[1, 128]], base=0, channel_multiplier=0), w=["iot_i"])
        P.op('pool', lambda e: e.iota(iop_i[:], [[0, 128]], base=0, channel_multiplier=1), w=["iop_i"])
        P.op('dve', lambda e: e.tensor_tensor(ident[:], iot_i[:], iop_i[:], ALU.is_equal), r=["iot_i", "iop_i"], w=["ident"])
        P.op('dve', lambda e: e.tensor_copy(identb[:], ident[:]), r=["ident"], w=["identb"])
        P.op('dve', lambda e: e.memset(ones_b[:], 1.0 / 128), w=["ones_b"])
        P.op('dve', lambda e: e.memset(onesD_b[:], 1.0 / 1024), w=["onesD_b"])
        P.op('dve', lambda e: e.memset(ones_f[:], 1.0), w=["ones_f"])

        stg = sb("stg", [128, 128], F32)
        rows = [(c, 8), (ada_b, 48), (norm1_g, 8), (lb_logits[0, :], 8), (lb_logits[1, :], 8), (hg_norm_g, 8),
                (conv_b, 8), (conv_ln_g, 8), (conv_ln_b, 8), (norm2_g, 8), (final_g, 8)]
        r0 = 0
        offs = []
        for apx, n in rows:
            offs.append(r0)
            P.dma('sp', lambda e, apx=apx, r0=r0, n=n: e.dma_start(out=stg[r0:r0 + n, :], in_=apx.rearrange("(j p) -> j p", p=128)),
                  w=["stg"])
            r0 += n
        assert r0 == 128
        (O_C, O_ADAB, O_N1G, O_LB0, O_LB1, O_HGG, O_CB, O_LNG, O_LNB, O_N2G, O_FG) = offs
        colp = sb("colp", [128, 128], F32)
        P.op('pe', lambda e: e.transpose(gp[0][:, 0:128], stg[:], ident[:]), r=["stg", "ident"], w=[GT[0]])
        P.op('dve', lambda e: e.tensor_copy(colp[:], gp[0][:, 0:128]), r=[GT[0]], w=["colp"])
        cw = sb("cw", [128, 256], F32)
        stg2 = sb("stg2", [128, 2, 128], F32)
        P.op('dve', lambda e: e.memset(stg2[:], 0.0), w=["stg2"])
        cwv = conv_w.rearrange("k (j p) -> (k j) p", p=128)
        P.dma('sp', lambda e: e.dma_start(out=stg2[:, 0, :], in_=cwv[0:128, :]), w=["stg2"])
        P.dma('sp', lambda e: e.dma_start(out=stg2[0:120, 1, :], in_=cwv[128:248, :]), w=["stg2"])
        for hh in range(2):
            P.op('pe', lambda e, hh=hh: e.transpose(gp[1][:, hh * 128:(hh + 1) * 128], stg2[:, hh, :], ident[:]),
                 r=["stg2", "ident"], w=[GT[1]])
        P.op('dve', lambda e: e.tensor_copy(cw[:], gp[1][:, 0:256]), r=[GT[1]], w=["cw"])

        sc_col = sb("sc_col", [128, 8], F32)
        ada_col = sb("ada_col", [128, 48], F32)
        NV = 12
        cvec = sb("cvec", [128, NV, 8], F32)
        phM = ExitStack()
        uT = sb("uT", [128, 8, S], BF16, phM)
        wg = [sb(f"wg{i}", [128, 8, 128], BF16, phM) for i in range(4)]
        wgf = [sb(f"wgf{i}", [128, 8, 128], F32, phM) for i in range(1)]
        oaT = sb("oaT", [128, 8, S], BF16, phM)
        NCB = 8
        cbuf = [oaT[:, i, :].rearrange("p (r d) -> p r d", r=2) for i in range(NCB)]
        tU = peer_u.rearrange("(c p r) d -> c p r d", p=128, r=2)
        tV = peer_v.rearrange("(c p r) d -> c p r d", p=128, r=2)
        tO = tabUV.rearrange("(c p r) d -> c p r d", p=128, r=2)
        jobs = [(tU, c, 0) for c in range(64)] + [(tV, c, 1024) for c in range(64)]

        def conv_store(n):
            tv_, c_, off_ = jobs[n]
            P.dma('pool', lambda e: e.dma_start(out=tO[c_][:, :, off_:off_ + 1024], in_=cbuf[n % NCB]), r=[f"cbuf{n % NCB}"], w=["tabUV"])
        for n, (tv_, c_, off_) in enumerate(jobs):
            P.dma('pool', lambda e, tv_=tv_, c_=c_, n=n: e.dma_start(out=cbuf[n % NCB], in_=tv_[c_]), w=[f"cbuf{n % NCB}"])
            if n >= 4:
                conv_store(n - 4)
        for n in range(len(jobs) - 4, len(jobs)):
            conv_store(n)

        P.op('act', lambda e: e.activation(sc_col[:], colp[:, O_C:O_C + 8], AF.Silu), r=["colp"], w=["sc_col"])

        with ExitStack() as st_ada:
            awt = [sb(f"awt{i}", [128, 8, 512], F32, st_ada) for i in range(2)]
            awv = ada_w.rearrange("(kc p) n -> p kc n", p=128)
            for g4 in range(12):
                bi = g4 % 2
                P.dma('sp', lambda e, g4=g4, bi=bi: e.dma_start(out=awt[bi][:], in_=awv[:, :, g4 * 512:(g4 + 1) * 512]),
                      w=[f"awt{bi}"])
                for gg in range(4):
                    col = g4 * 4 + gg
                    for kc in range(8):
                        P.op('pe', lambda e, bi=bi, gg=gg, kc=kc, col=col: e.matmul(
                            gp[2][:, col:col + 1], awt[bi][:, kc, gg * 128:(gg + 1) * 128], sc_col[:, kc:kc + 1],
                            start=(kc == 0), stop=(kc == 7)), r=[f"awt{bi}", "sc_col"], w=[GT[2]])
            P.op('dve', lambda e: e.tensor_tensor(ada_col[:], gp[2][:, 0:48], colp[:, O_ADAB:O_ADAB + 48], ALU.add),
                 r=[GT[2], "colp"], w=["ada_col"])
        P.barrier(skip_pool_dma=True)
        (V_W1, V_SH1, V_W2, V_SH2, V_G1, V_G2, V_LB, V_OML, V_NOML, V_FG, V_TMP, V_TMP2) = range(NV)
        A_SH1, A_SC1, A_G1, A_SH2, A_SC2, A_G2 = [ada_col[:, i * 8:(i + 1) * 8] for i in range(6)]
        R_ = ["ada_col", "colp", "cvec"]
        P.op('dve', lambda e: e.scalar_tensor_tensor(cvec[:, V_W1, :], A_SC1, 1.0, colp[:, O_N1G:O_N1G + 8], ALU.add, ALU.mult), r=R_, w=["cvec"])
        P.op('dve', lambda e: e.tensor_copy(cvec[:, V_SH1, :], A_SH1), r=R_, w=["cvec"])
        P.op('dve', lambda e: e.scalar_tensor_tensor(cvec[:, V_W2, :], A_SC2, 1.0, colp[:, O_N2G:O_N2G + 8], ALU.add, ALU.mult), r=R_, w=["cvec"])
        P.op('dve', lambda e: e.tensor_copy(cvec[:, V_SH2, :], A_SH2), r=R_, w=["cvec"])
        P.op('dve', lambda e: e.tensor_copy(cvec[:, V_G1, :], A_G1), r=R_, w=["cvec"])
        P.op('dve', lambda e: e.tensor_copy(cvec[:, V_G2, :], A_G2), r=R_, w=["cvec"])
        P.op('dve', lambda e: e.tensor_copy(cvec[:, V_FG, :], colp[:, O_FG:O_FG + 8]), r=R_, w=["cvec"])
        P.op('dve', lambda e: e.tensor_tensor(cvec[:, V_TMP, :], colp[:, O_LB0:O_LB0 + 8], colp[:, O_LB1:O_LB1 + 8], ALU.subtract), r=R_, w=["cvec"])
        P.op('act', lambda e: e.activation(cvec[:, V_LB, :], cvec[:, V_TMP, :], AF.Sigmoid), r=["cvec"], w=["cvec"])
        P.op('dve', lambda e: e.tensor_scalar(cvec[:, V_OML, :], cvec[:, V_LB, :], -1.0, 1.0, ALU.mult, ALU.add), r=["cvec"], w=["cvec"])
        P.op('dve', lambda e: e.tensor_scalar(cvec[:, V_NOML, :], cvec[:, V_OML, :], -1.0, None, ALU.mult), r=["cvec"], w=["cvec"])

        if stage == 0:
            P.dma('sp', lambda e: e.dma_start(out=dbg[0:128, 0:NV * 8], in_=cvec[:].rearrange("p a b -> p (a b)")), r=["cvec"], w=["dbg"])
            P.dma('sp', lambda e: e.dma_start(out=dbg[128:256, :], in_=rowv[:, 0, :]), r=["rowv"], w=["dbg"])
            P.dma('sp', lambda e: e.dma_start(out=dbg[256:384, 0:256], in_=cw[:]), r=["cw"], w=["dbg"])
            P.dma('sp', lambda e: e.dma_start(out=dbg[384:512, 0:48], in_=ada_col[:]), r=["ada_col"], w=["dbg"])
            P.finish(["dbg"])
            return nc


        ph1 = ExitStack()
        xt = [sb(f"xt{i}", [128, 1024], F32, ph1) for i in range(2)]
        xn = [sb(f"xn{i}", [128, 1024], F32, ph1) for i in range(2)]
        junk = sb("junk", [128, 1024], F32, ph1)
        st1 = sb("st1", [128, 2, 4], F32, ph1)
        for i in range(NT):
            b = i % 2
            P.dma('sp', lambda e, i=i, b=b: e.dma_start(out=xt[b][:], in_=x[i * 128:(i + 1) * 128, :]), w=[f"xt{b}"])
            P.op('dve', lambda e, b=b: e.scalar_tensor_tensor(junk[:], xt[b][:], 1.0, xt[b][:], ALU.mult, ALU.mult,
                                                              accum_out=st1[:, b, 0:1]), r=[f"xt{b}"], w=["junk", f"st1{b}"])
            P.op('act', lambda e, b=b: e.activation(st1[:, b, 1:2], st1[:, b, 0:1], AF.Sqrt, scale=1.0 / D, bias=EPS),
                 r=[f"st1{b}"], w=[f"st1{b}"])
            P.op('dve', lambda e, b=b: e.reciprocal(st1[:, b, 2:3], st1[:, b, 1:2]), r=[f"st1{b}"], w=[f"st1{b}"])
            P.op('dve', lambda e, b=b: e.tensor_scalar(xn[b][:], xt[b][:], st1[:, b, 2:3], None, ALU.mult),
                 r=[f"xt{b}", f"st1{b}"], w=[f"xn{b}"])
            for j in range(8):
                pb = j // 4
                P.op('pe', lambda e, b=b, j=j, pb=pb: e.transpose(gp[pb][:, (j % 4) * 128:(j % 4 + 1) * 128],
                                                               xn[b][:, j * 128:(j + 1) * 128], ident[:]),
                     r=[f"xn{b}", "ident"], w=[GT[pb]])
            for j in range(8):
                pb = j // 4
                P.op('act', lambda e, i=i, j=j, pb=pb: e.activation(
                    uT[:, j, i * 128:(i + 1) * 128], gp[pb][:, (j % 4) * 128:(j % 4 + 1) * 128], AF.Identity,
                    scale=cvec[:, V_W1, j:j + 1], bias=cvec[:, V_SH1, j:j + 1]), r=[GT[pb], "cvec"], w=["uT"])
        ph1.close()
        P.barrier(skip_pool_dma=True)

        NWG = 4
        wctr = [0]

        def load_wg(W, col0):
            b = wctr[0] % NWG
            bf = 0
            wctr[0] += 1
            Wv = W.rearrange("(kc p) n -> p kc n", p=128)
            P.dma('sp', lambda e: e.dma_start(out=wgf[bf][:], in_=Wv[:, :, col0:col0 + 128]), w=[f"wgf{bf}"])
            P.op('act', lambda e: e.copy(wg[b][:], wgf[bf][:]), r=[f"wgf{bf}"], w=[f"wg{b}"])
            return wg[b], f"wg{b}"

        zctr = [0]

        def zmm(wt, wtok, half):
            b = zctr[0] % 2
            zctr[0] += 1
            for tg in range(2):
                t0 = half * 1024 + tg * 512
                for kc in range(8):
                    P.op('pe', lambda e, tg=tg, kc=kc, t0=t0: e.matmul(zp[b][:, tg * 512:(tg + 1) * 512], wt[:, kc, :],
                                                                    uT[:, kc, t0:t0 + 512], start=(kc == 0), stop=(kc == 7)),
                         r=[wtok, "uT"], w=[ZT[b][tg]])
            return b

        if stage == 1:
            dbt = sb("dbt", [128, 1024], F32)
            for j in range(8):
                P.op('dve', lambda e, j=j: e.tensor_copy(dbt[:], uT[:, j, 0:1024]), r=["uT"], w=["dbt"])
                P.dma('sp', lambda e, j=j: e.dma_start(out=dbg[j * 128:(j + 1) * 128, :], in_=dbt[:]), r=["dbt"], w=["dbg"])
            P.finish(["dbg"])
            return nc

        cvT = sb("cvT", [128, 8, S], BF16, phM)
        phB = ExitStack()
        glu = sb("glu", [128, 8, 32 + S], BF16, phB)
        PADL = 32
        dgc = [sb(f"dgc{i}", [128, 31, 128], BF16, phB) for i in range(2)]
        sgb = [sb(f"sgb{i}", [128, 1024], F32, phB) for i in range(2)]
        sqt = [sb(f"sqt{i}", [128, 512], BF16, phB) for i in range(2)]
        lnt = [sb(f"lnt{i}", [128, 512], F32, phB) for i in range(4)]
        for j in range(8):
            P.op('dve', lambda e, j=j: e.memset(glu[:, j, 0:PADL], 0.0), w=[f"glu{j}"])
        def b_loads(j):
            return load_wg(w_in, (32 + j) * 128), load_wg(w_in, (40 + j) * 128)

        def b_diag(j):
            dj = dgc[j % 2]
            for kk in range(31):
                P.op('dve', lambda e, kk=kk, dj=dj: e.tensor_scalar(dj[:, kk, :], ident[:], cw[:, kk * 8 + j:kk * 8 + j + 1], None, ALU.mult),
                     r=["ident", "cw"], w=[f"dgc{j % 2}"])

        wnext = b_loads(0)
        b_diag(0)
        for j in range(8):
            (wa_, ta_), (wb_, tb_) = wnext
            if j + 1 < 8:
                wnext = b_loads(j + 1)
            for half in range(2):
                bb = zmm(wb_, tb_, half)
                sb_ = sgb[half]
                P.op('act', lambda e, bb=bb, sb_=sb_: e.activation(sb_[:], zp[bb][:], AF.Sigmoid), r=ZT[bb], w=[f"sgb{half}"])
                ba = zmm(wa_, ta_, half)
                P.op('dve', lambda e, ba=ba, sb_=sb_, j=j, half=half: e.tensor_tensor(
                    glu[:, j, PADL + half * 1024:PADL + (half + 1) * 1024], zp[ba][:], sb_[:], ALU.mult),
                    r=ZT[ba] + [f"sgb{half}"], w=[f"glu{j}"])
            if j + 1 < 8:
                b_diag(j + 1)
            dj = dgc[j % 2]
            for tg in range(4):
                t0 = tg * 512
                pb = tg % 2
                for kk in range(31):
                    P.op('pe', lambda e, j=j, kk=kk, pb=pb, t0=t0, dj=dj: e.matmul(
                        gp[pb][:], dj[:, kk, :], glu[:, j, PADL - 30 + t0 + kk:PADL - 30 + t0 + kk + 512],
                        start=(kk == 0), stop=(kk == 30)), r=[f"dgc{j % 2}", f"glu{j}"], w=[GT[pb]])
                P.op('act', lambda e, j=j, pb=pb, t0=t0: e.activation(cvT[:, j, t0:t0 + 512], gp[pb][:], AF.Identity,
                                                                    bias=colp[:, O_CB + j:O_CB + j + 1]), r=[GT[pb], "colp"], w=[f"cvT{tg}"])
        if stage == 21:
            dbt = sb("dbt", [128, 1024], F32, phB)
            for j in range(8):
                P.op('dve', lambda e, j=j: e.tensor_copy(dbt[:], glu[:, j, PADL:PADL + 1024]), r=[f"glu{j}"], w=["dbt"])
                P.dma('sp', lambda e, j=j: e.dma_start(out=dbg[j * 128:(j + 1) * 128, :], in_=dbt[:]), r=["dbt"], w=["dbg"])
            for j in range(8):
                P.op('dve', lambda e, j=j: e.tensor_copy(dbt[:], cvT[:, j, 0:1024]), r=["cvT0", "cvT1"], w=["dbt"])
                P.dma('sp', lambda e, j=j: e.dma_start(out=dbg[1024 + j * 128:1024 + (j + 1) * 128, :], in_=dbt[:]), r=["dbt"], w=["dbg"])
            P.finish(["dbg"])
            phB.close()
            return nc
        for tg in range(4):
            t0 = tg * 512
            for j in range(8):
                P.op('pe', lambda e, j=j, t0=t0: e.matmul(gp[2][:], onesD_b[:], cvT[:, j, t0:t0 + 512], start=(j == 0), stop=(j == 7)),
                     r=["onesD_b", f"cvT{tg}"], w=[GT[2]])
            for j in range(8):
                P.op('act', lambda e, j=j, t0=t0: e.activation(sqt[j % 2][:], cvT[:, j, t0:t0 + 512], AF.Square), r=[f"cvT{tg}"], w=[f"sqt{j % 2}"])
                P.op('pe', lambda e, j=j, t0=t0: e.matmul(gp[3][:], onesD_b[:], sqt[j % 2][:], start=(j == 0), stop=(j == 7)),
                     r=["onesD_b", f"sqt{j % 2}"], w=[GT[3]])
            mean_, var_, rstd_, tmp_ = lnt
            P.op('act', lambda e: e.copy(mean_[:], gp[2][:]), r=[GT[2]], w=["lnt0"])
            P.op('dve', lambda e: e.tensor_tensor(var_[:], mean_[:], mean_[:], ALU.mult), r=["lnt0"], w=["lnt1"])
            P.op('dve', lambda e: e.tensor_tensor(var_[:], gp[3][:], var_[:], ALU.subtract), r=[GT[3], "lnt1"], w=["lnt1"])
            P.op('dve', lambda e: e.tensor_scalar(var_[:], var_[:], 0.0, None, ALU.max), r=["lnt1"], w=["lnt1"])
            P.op('act', lambda e: e.activation(rstd_[:], var_[:], AF.Sqrt, bias=EPS), r=["lnt1"], w=["lnt2"])
            P.op('dve', lambda e: e.reciprocal(rstd_[:], rstd_[:]), r=["lnt2"], w=["lnt2"])
            for j in range(8):
                P.op('dve', lambda e, j=j, t0=t0: e.tensor_tensor(tmp_[:], cvT[:, j, t0:t0 + 512], mean_[:], ALU.subtract),
                     r=[f"cvT{tg}", "lnt0"], w=["lnt3"])
                P.op('dve', lambda e: e.tensor_tensor(tmp_[:], tmp_[:], rstd_[:], ALU.mult), r=["lnt3", "lnt2"], w=["lnt3"])
                P.op('act', lambda e, j=j, t0=t0: e.activation(cvT[:, j, t0:t0 + 512], tmp_[:], AF.Silu,
                                                             scale=colp[:, O_LNG + j:O_LNG + j + 1], bias=colp[:, O_LNB + j:O_LNB + j + 1]),
                     r=["lnt3", "colp"], w=[f"cvT{tg}"])
        phB.close()
        P.barrier()

        if stage == 2:
            dbt = sb("dbt", [128, 1024], F32)
            for j in range(8):
                P.op('dve', lambda e, j=j: e.tensor_copy(dbt[:], cvT[:, j, 0:1024]), r=["cvT0","cvT1","cvT2","cvT3"], w=["dbt"])
                P.dma('sp', lambda e, j=j: e.dma_start(out=dbg[j * 128:(j + 1) * 128, :], in_=dbt[:]), r=["dbt"], w=["dbg"])
            for j in range(8):
                P.op('dve', lambda e, j=j: e.tensor_copy(dbt[:], cvT[:, j, 1024:2048]), r=["cvT0","cvT1","cvT2","cvT3"], w=["dbt"])
                P.dma('sp', lambda e, j=j: e.dma_start(out=dbg[1024 + j * 128:1024 + (j + 1) * 128, :], in_=dbt[:]), r=["dbt"], w=["dbg"])
            P.finish(["dbg"])
            return nc


        phA = ExitStack()
        hA = sb("hA", [128, S], F32, phA)
        hB = sb("hB", [128, S], F32, phA)
        hC = [sb(f"hC{i}", [128, S], F32, phA) for i in range(2)]
        msk = sb("msk", [128, S], BF16, phA)
        qt = [sb(f"qt{i}", [128, S], BF16, phA) for i in range(2)]
        kt = [sb(f"kt{i}", [128, S], BF16, phA) for i in range(2)]
        vT = [sb(f"vT{i}", [128, S], BF16, phA) for i in range(2)]
        sg = [sb(f"sg{i}", [128, S], BF16, phA) for i in range(2)]
        vtok2 = [sb(f"vtok{i}", [32, 8, 128], BF16, phA) for i in range(2)]
        ktok = sb("ktok", [32, 8, 128], BF16, phA)
        Sall = sb("Sall", [128, 9, 128], BF16, phA)
        Rst2 = [sb(f"Rst{i}", [128, 128], F32, phA) for i in range(2)]
        nhalf = sb("nhalf", [128, 256], F32, phA)
        msb = sb("msb", [128, 256], F32, phA)
        PT2 = [sb(f"PT{i}", [32, 8, 32], BF16, phA) for i in range(2)]
        tri = sb("tri", [32, 32], F32, phA)
        osq = sb("osq", [128, 256], BF16, phA)
        lsc = sb("lsc", [128, 3, 256], F32, phA)
        rs_, tmpo, hOq = lsc[:, 0, :], lsc[:, 1, :], lsc[:, 2, :]
        hOi = hA[:].bitcast(I32)
        P.op('pool', lambda e: e.iota(hOi, [[1, S]], base=0, channel_multiplier=0), w=["hA"])
        P.op('dve', lambda e: e.tensor_scalar(hOi, hOi, 31, None, ALU.bitwise_and), r=["hA"], w=["hA"])
        P.op('dve', lambda e: e.tensor_scalar(msk[:], hOi, 0.0, None, ALU.is_gt), r=["hA"], w=["msk"])
        P.op('dve', lambda e: e.tensor_tensor(tri[:], iot_i[0:32, 0:32], iop_i[0:32, 0:32], ALU.is_ge), r=["iot_i", "iop_i"], w=["tri"])
        gpb = [g_[:].bitcast(BF16) for g_ in gp]
        P.op('dve', lambda e: e.memset(nhalf[:], -0.5), w=["nhalf"])

        def zmm0(wt, wtok, half):
            for tg in range(2):
                t0 = half * 1024 + tg * 512
                for kc in range(8):
                    P.op('pe', lambda e, tg=tg, kc=kc, t0=t0: e.matmul(zp[0][:, tg * 512:(tg + 1) * 512], wt[:, kc, :],
                                                                    uT[:, kc, t0:t0 + 512], start=(kc == 0), stop=(kc == 7)),
                         r=[wtok, "uT"], w=[ZT[0][tg]])

        def prologue(h):
            p = h % 2
            HC, QT, KT, VT, SG = hC[p], qt[p], kt[p], vT[p], sg[p]
            wf_, tf_ = load_wg(w_in, (8 + h) * 128)
            wq_, tq_ = load_wg(w_in, h * 128)
            wi_, ti_ = load_wg(w_in, (16 + h) * 128)
            wg_, tg_ = load_wg(w_in, (24 + h) * 128)
            for half in range(2):
                zmm0(wf_, tf_, half)
                P.op('act', lambda e, half=half: e.activation(hA[:, half * 1024:(half + 1) * 1024], zp[0][:], AF.Sigmoid),
                     r=ZT[0], w=["hA"])
                yield
            P.op('act', lambda e: e.activation(hB[:], hA[:], AF.Ln, scale=cvec[:, V_OML, h:h + 1], bias=cvec[:, V_LB, h:h + 1]),
                 r=["hA", "cvec"], w=["hB"])
            P.op('dve', lambda e: e.tensor_scalar(hA[:], hA[:], cvec[:, V_NOML, h:h + 1], cvec[:, V_OML, h:h + 1], ALU.mult, ALU.add),
                 r=["hA", "cvec"], w=["hA"])
            P.op('dve', lambda e: e.tensor_tensor_scan(HC[:], msk[:], hB[:], 0.0, ALU.mult, ALU.add), r=["msk", "hB"], w=[f"hC{p}"])
            P.op('act', lambda e: e.activation(hB[:], HC[:], AF.Exp, scale=-1.0), r=[f"hC{p}"], w=["hB"])
            P.op('dve', lambda e: e.tensor_tensor(KT[:], hA[:], hB[:], ALU.mult), r=["hA", "hB"], w=[f"kt{p}"])
            P.op('act', lambda e: e.activation(HC[:], HC[:], AF.Exp), r=[f"hC{p}", "hB"], w=[f"hC{p}"])
            for half in range(2):
                zmm0(wq_, tq_, half)
                P.op('dve', lambda e, half=half: e.tensor_tensor(QT[:, half * 1024:(half + 1) * 1024], zp[0][:],
                                                               HC[:, half * 1024:(half + 1) * 1024], ALU.mult),
                     r=ZT[0] + [f"hC{p}"], w=[f"qt{p}"])
                yield
            for half in range(2):
                zmm0(wi_, ti_, half)
                P.op('act', lambda e, half=half: e.copy(VT[:, half * 1024:(half + 1) * 1024], zp[0][:]), r=ZT[0], w=[f"vT{p}"])
                yield
            for half in range(2):
                zmm0(wg_, tg_, half)
                P.op('act', lambda e, half=half: e.activation(SG[:, half * 1024:(half + 1) * 1024], zp[0][:], AF.Silu),
                     r=ZT[0], w=[f"sg{p}"])
                yield

        def chunk_loop(h, nxt):
            p = h % 2
            HC, QT, KT, VT, SG = hC[p], qt[p], kt[p], vT[p], sg[p]
            tHC, tQT, tKT, tVT, tSG = f"hC{p}", f"qt{p}", f"kt{p}", f"vT{p}", f"sg{p}"
            P.op('dve', lambda e: e.memset(Sall[:, 0, :], 0.0), w=["Sall"])

            def g_T_SC(qd):
                c0 = qd * 8
                for cc in range(8):
                    t0 = (c0 + cc) * 32
                    P.op('pe', lambda e, cc=cc, t0=t0: e.transpose(gpb[0][0:32, cc * 128:(cc + 1) * 128],
                                                                 KT[:, t0:t0 + 32], identb[:]), r=[tKT, "identb"], w=[GT[0]])
                    P.op('pe', lambda e, cc=cc, t0=t0: e.transpose(gpb[2][0:32, cc * 128:(cc + 1) * 128],
                                                                 VT[:, t0:t0 + 32], identb[:]), r=[tVT, "identb"], w=[GT[2]])
                for cc in range(8):
                    t0 = (c0 + cc) * 32
                    P.op('pe', lambda e, cc=cc, t0=t0: e.matmul(zp[1][0:32, cc * 32:(cc + 1) * 32], KT[:, t0:t0 + 32], QT[:, t0:t0 + 32],
                                                              start=True, stop=True), r=[tKT, tQT], w=[ZT[1][0]])

            def g_evac_mask(qd):
                pv = qd % 2
                P.op('act', lambda e: e.copy(ktok[:].rearrange("p a b -> p (a b)"), gpb[0][0:32, :]), r=[GT[0]], w=["ktok"])
                P.op('dve', lambda e: e.tensor_copy(vtok2[pv][:].rearrange("p a b -> p (a b)"), gpb[2][0:32, :]), r=[GT[2]], w=[f"vtok{pv}"])
                P.op('dve', lambda e: e.tensor_tensor(PT2[pv][:], zp[1][0:32, 0:256].rearrange("p (c t) -> p c t", t=32),
                                                      tri[:].unsqueeze(1).to_broadcast([32, 8, 32]), ALU.mult),
                     r=[ZT[1][0], "tri"], w=[f"PT{pv}"])

            def norm_tail(qd):
                q0 = qd * 256
                P.op('pe', lambda e: e.matmul(zp[1][:, 768:1024], ones_b[:], osq[:], start=True, stop=True), r=["ones_b", "osq"], w=[ZT[1][1]])
                P.op('act', lambda e: e.activation(rs_, zp[1][:, 768:1024], AF.Sqrt, bias=EPS), r=[ZT[1][1]], w=["rs_"])
                P.op('dve', lambda e: e.reciprocal(rs_, rs_), r=["rs_"], w=["rs_"])
                P.op('dve', lambda e: e.tensor_tensor(tmpo, hOq, rs_, ALU.mult), r=["hOq", "rs_"], w=["tmpo"])
                P.op('dve', lambda e: e.scalar_tensor_tensor(oaT[:, h, q0:q0 + 256], tmpo, colp[:, O_HGG + h:O_HGG + h + 1],
                                                             SG[:, q0:q0 + 256], ALU.mult, ALU.mult),
                     r=["tmpo", "colp", tSG], w=["oaT"])

            g_T_SC(0)
            g_evac_mask(0)
            for qd in range(8):
                c0 = qd * 8
                pv = qd % 2
                VTK, PTK = vtok2[pv], PT2[pv]
                for cc in range(8):
                    P.op('pe', lambda e, cc=cc: e.matmul(gp[1 + 2 * (cc // 4)][:, (cc % 4) * 128:(cc % 4 + 1) * 128], ktok[:, cc, :], VTK[:, cc, :],
                                                       start=True, stop=True), r=["ktok", f"vtok{pv}"], w=[GT[1 + 2 * (cc // 4)]])
                if qd < 7:
                    g_T_SC(qd + 1)
                for cc in range(8):
                    c = c0 + cc
                    kvap = gp[1 + 2 * (cc // 4)][:, (cc % 4) * 128:(cc % 4 + 1) * 128]
                    Rc, Rp = Rst2[c % 2], Rst2[(c + 1) % 2]
                    if c == 0:
                        P.op('dve', lambda e, kvap=kvap, Rc=Rc: e.tensor_copy(Rc[:], kvap), r=[GT[1 + 2 * (cc // 4)]], w=[f"Rst{c % 2}"])
                    else:
                        dprev = HC[:, 32 * (c - 1) + 31:32 * (c - 1) + 32]
                        P.op('dve', lambda e, kvap=kvap, dprev=dprev, Rc=Rc, Rp=Rp: e.scalar_tensor_tensor(Rc[:], Rp[:], dprev, kvap, ALU.mult, ALU.add),
                             r=[GT[1 + 2 * (cc // 4)], f"Rst{(c + 1) % 2}", tHC], w=[f"Rst{c % 2}"])
                    dcur = HC[:, 32 * c + 31:32 * c + 32]
                    P.op('act', lambda e, cc=cc, dcur=dcur, Rc=Rc: e.activation(Sall[:, cc + 1, :], Rc[:], AF.Identity, scale=dcur),
                         r=[f"Rst{c % 2}", tHC], w=["Sall"])
                if qd < 7:
                    g_evac_mask(qd + 1)
                if qd >= 1:
                    norm_tail(qd - 1)
                if nxt is not None:
                    next(nxt, None)
                for cc in range(8):
                    t0 = (c0 + cc) * 32
                    P.op('pe', lambda e, cc=cc: e.matmul(zp[1][:, 512 + cc * 32:512 + (cc + 1) * 32], VTK[:, cc, :], PTK[:, cc, :],
                                                       start=True, stop=False), r=[f"vtok{pv}", f"PT{pv}"], w=[ZT[1][1]])
                    P.op('pe', lambda e, cc=cc, t0=t0: e.matmul(zp[1][:, 512 + cc * 32:512 + (cc + 1) * 32], Sall[:, cc, :], QT[:, t0:t0 + 32],
                                                              start=False, stop=True), r=["Sall", tQT], w=[ZT[1][1]])
                if qd < 7:
                    P.op('dve', lambda e: e.tensor_copy(Sall[:, 0, :], Sall[:, 8, :]), r=["Sall"], w=["Sall"])
                P.op('act', lambda e: e.copy(hOq, zp[1][:, 512:768]), r=[ZT[1][1]], w=["hOq"])
                P.op('act', lambda e: e.activation(osq[:], zp[1][:, 512:768], AF.Square), r=[ZT[1][1]], w=["osq"])
            norm_tail(7)
            if nxt is not None:
                for _ in nxt:
                    pass

        for _ in prologue(0):
            pass
        for h in range(8):
            chunk_loop(h, prologue(h + 1) if h + 1 < 8 else None)
        phA.close()
        P.barrier()
        mergedT = sb("mergedT", [128, 8, S], BF16, phM)

        phG = ExitStack()
        m1 = sb("m1", [128, 1024], F32, phG)
        m2 = sb("m2", [128, 1024], F32, phG)
        CVT = ["cvT0", "cvT1", "cvT2", "cvT3"]
        for j in range(8):
            wa_, ta_ = load_wg(w_a, j * 128)
            wb_, tb_ = load_wg(w_b, j * 128)
            wga_, tga_ = load_wg(w_in, (48 + j) * 128)
            wgb_, tgb_ = load_wg(w_in, (56 + j) * 128)
            for half in range(2):
                b = zmm(wga_, tga_, half)
                P.op('act', lambda e, b=b: e.activation(m1[:], zp[b][:], AF.Sigmoid), r=ZT[b], w=["m1"])
                b = zmm(wgb_, tgb_, half)
                P.op('act', lambda e, b=b: e.activation(m2[:], zp[b][:], AF.Sigmoid), r=ZT[b], w=["m2"])
                for tg in range(2):
                    t0 = half * 1024 + tg * 512
                    for kc in range(8):
                        P.op('pe', lambda e, tg=tg, kc=kc, t0=t0: e.matmul(gp[tg][:], wa_[:, kc, :], oaT[:, kc, t0:t0 + 512],
                                                                        start=(kc == 0), stop=(kc == 7)), r=[ta_, "oaT"], w=[GT[tg]])
                    for kc in range(8):
                        P.op('pe', lambda e, tg=tg, kc=kc, t0=t0: e.matmul(gp[2 + tg][:], wb_[:, kc, :], cvT[:, kc, t0:t0 + 512],
                                                                        start=(kc == 0), stop=(kc == 7)), r=[tb_] + CVT, w=[GT[2 + tg]])
                    P.op('dve', lambda e, tg=tg: e.tensor_tensor(m1[:, tg * 512:(tg + 1) * 512], gp[tg][:], m1[:, tg * 512:(tg + 1) * 512], ALU.mult),
                         r=[GT[tg], "m1"], w=["m1"])
                    P.op('dve', lambda e, tg=tg: e.tensor_tensor(m2[:, tg * 512:(tg + 1) * 512], gp[2 + tg][:], m2[:, tg * 512:(tg + 1) * 512], ALU.mult),
                         r=[GT[2 + tg], "m2"], w=["m2"])
                P.op('dve', lambda e, j=j, half=half: e.tensor_tensor(mergedT[:, j, half * 1024:(half + 1) * 1024], m1[:], m2[:], ALU.add),
                     r=["m1", "m2"], w=["mergedT"])
        phG.close()
        P.barrier()

        def rowform(dst_ap, vv, stack):
            dgl = [sb(f"dg{vv}_{i}", [128, 128], F32, stack) for i in range(2)]
            for j in range(8):
                b_ = j % 2
                P.op('dve', lambda e, b_=b_, j=j: e.tensor_scalar(dgl[b_][:], ident[:], cvec[:, vv, j:j + 1], None, ALU.mult),
                     r=["ident", "cvec"], w=[f"dgl{vv}_{b_}"])
                pb = 2 + b_
                P.op('pe', lambda e, b_=b_, pb=pb: e.matmul(gp[pb][:, 0:128], ones_f[:], dgl[b_][:], start=True, stop=True),
                     r=["ones_f", f"dgl{vv}_{b_}"], w=[GT[pb]])
                P.op('act', lambda e, pb=pb, j=j: e.copy(dst_ap[:, j * 128:(j + 1) * 128], gp[pb][:, 0:128]), r=[GT[pb]], w=["rowv"])

        phY = ExitStack()
        rg1 = sb("rg1", [128, 1024], F32, phY)
        rowform(rg1, V_G1, phY)
        wout = sb("wout", [128, 8, 1024], BF16, phY)
        wov = w_out.rearrange("(kc p) n -> p kc n", p=128)
        for kc in range(8):
            P.dma('pool', lambda e, kc=kc: e.dma_start(out=wout[:, kc, :], in_=wov[:, kc, :]), w=["wout"])
        xta = [sb(f"xta{i}", [128, 1024], F32, phY) for i in range(2)]
        h1a = [sb(f"h1a{i}", [128, 1024], F32, phY) for i in range(2)]
        for i in range(NT):
            b = i % 2
            P.dma('sp', lambda e, i=i, b=b: e.dma_start(out=xta[b][:], in_=x[i * 128:(i + 1) * 128, :]), w=[f"xta{b}"])
            for ng in range(2):
                for kc in range(8):
                    P.op('pe', lambda e, i=i, b=b, ng=ng, kc=kc: e.matmul(zp[b][:, ng * 512:(ng + 1) * 512], mergedT[:, kc, i * 128:(i + 1) * 128],
                                                                        wout[:, kc, ng * 512:(ng + 1) * 512], start=(kc == 0), stop=(kc == 7)),
                         r=["mergedT", "wout"], w=[ZT[b][ng]])
            P.op('dve', lambda e, b=b: e.tensor_tensor(h1a[b][:], zp[b][:], rg1[:], ALU.mult), r=ZT[b] + ["rowv"], w=[f"h1a{b}"])
            P.op('dve', lambda e, b=b: e.tensor_tensor(h1a[b][:], h1a[b][:], xta[b][:], ALU.add), r=[f"h1a{b}", f"xta{b}"], w=[f"h1a{b}"])
            P.dma('sp', lambda e, i=i, b=b: e.dma_start(out=h1s[i * 128:(i + 1) * 128, :], in_=h1a[b][:]), r=[f"h1a{b}"], w=["h1s"])
        phY.close()
        phM.close()
        P.barrier()

        rowv = sb("rowv", [128, 2, 1024], F32)
        rowform(rowv[:, 0, :], V_G2, ES)
        rowform(rowv[:, 1, :], V_FG, ES)
        RG2, RFG = rowv[:, 0, :], rowv[:, 1, :]
        wqb = sb("wqb", [128, 8, 2048], BF16)
        keysT = sb("keysT", [128, 16, 128], BF16)
        s_ = sb("s_", [128, 16, 128], F32)
        wst = s_[:].rearrange("p a b -> p (a b)")
        wqv = peer_wq.rearrange("(kc p) n -> p kc n", p=128)
        for kc in range(8):
            P.dma('pool', lambda e, kc=kc: e.dma_start(out=wqb[:, kc, :], in_=wqv[:, kc, :]), w=["wqb"])
        kv_ = peer_keys.rearrange("g n d -> n g d")
        P.dma('sp', lambda e: e.dma_start(out=s_[:], in_=kv_), w=["s_"])
        for g in range(16):
            pb = g % 4
            P.op('pe', lambda e, g=g, pb=pb: e.transpose(gp[pb][:, 0:128], wst[:, g * 128:(g + 1) * 128], ident[:]),
                 r=["s_", "ident"], w=[GT[pb]])
            P.op('act', lambda e, g=g, pb=pb: e.copy(keysT[:, g, :], gp[pb][:, 0:128]), r=[GT[pb]], w=["keysT"])

        h1 = [sb(f"h1_{i}", [128, 1024], F32) for i in range(2)]
        u2 = [sb(f"u2_{i}", [128, 1024], BF16) for i in range(2)]
        prodb = [sb(f"prodb{i}", [128, 1024], BF16) for i in range(2)]
        xn2 = sb("xn2", [128, 1024], F32)
        jk = sb("jk", [128, 1024], BF16)
        fin = sb("fin", [128, 1024], F32)
        u2T = sb("u2T", [128, 8, 128], BF16)
        qT = sb("qT", [128, 16, 128], BF16)
        s2_ = sb("s2_", [128, 16, 128], F32)
        sc_ = sb("sc_", [128, 16, 16], F32)
        idx_ = sb("idx_", [128, 16, 16], U32)
        idxf = sb("idxf", [128, 16, 16], F32)
        cand = s_[:].rearrange("p a b -> p (a b)").rearrange("p (h c) -> p h c", c=256)
        cand2 = s2_[:].rearrange("p a b -> p (a b)").rearrange("p (h c) -> p h c", c=256)
        oh = s2_[:].rearrange("p a b -> p (a b)").rearrange("p (h k a) -> p h k a", k=16, a=16)
        top_ = sb("top_", [128, 8, 16], F32)
        pos_ = sb("pos_", [128, 8, 16], U32)
        pa_ = sb("pa_", [128, 8, 16], U32)
        paf = sb("paf", [128, 8, 16], F32)
        pbf = sb("pbf", [128, 8, 16], F32)
        isel = sb("isel", [128, 128], F32)
        jsel = sb("jsel", [128, 128], F32)
        eidx = [sb(f"eidx{i}", [128, 128], U32) for i in range(2)]
        gate = [sb(f"gate{i}", [128, 128], F32) for i in range(2)]
        gsum = sb("gsum", [128, 8], F32)
        act_ = sb("act_", [128, 128], F32)
        gl_ = sb("gl_", [128, 128], F32)
        st2 = sb("st2", [128, 8], F32)
        st3 = sb("st3", [128, 8], F32)
        io16 = sb("io16", [128, 16], F32)
        NGB = 22
        GB = [sb(f"GB{i}", [128, 2048], BF16) for i in range(NGB)]
        dgs = [sb(f"dgs{i}", [128, 128], BF16) for i in range(4)]
        P.op('dve', lambda e: e.tensor_copy(io16[:], iot_i[:, 0:16]), r=["iot_i"], w=["io16"])
        NEG = -1.0e30

        def front(i):
            b = i % 2
            H1, U2, EIDX, GATE = h1[b], u2[b], eidx[b], gate[b]
            g3 = GATE[:].rearrange("p (h k) -> p h k", k=16)
            P.dma('sp', lambda e: e.dma_start(out=H1[:], in_=h1s[i * 128:(i + 1) * 128, :]), r=["h1s"], w=[f"h1_{b}"])
            yield
            P.op('dve', lambda e: e.scalar_tensor_tensor(xn2[:], H1[:], 1.0, H1[:], ALU.mult, ALU.mult, accum_out=st2[:, 0:1]),
                 r=[f"h1_{b}"], w=["xn2", "st2"])
            yield
            P.op('act', lambda e: e.activation(st2[:, 1:2], st2[:, 0:1], AF.Sqrt, scale=1.0 / D, bias=EPS), r=["st2"], w=["st2"])
            P.op('dve', lambda e: e.reciprocal(st2[:, 2:3], st2[:, 1:2]), r=["st2"], w=["st2"])
            yield
            P.op('dve', lambda e: e.tensor_scalar(xn2[:], H1[:], st2[:, 2:3], None, ALU.mult), r=[f"h1_{b}", "st2"], w=["xn2"])
            yield
            yield
            for j in range(8):
                pb = j // 4
                P.op('pe', lambda e, j=j, pb=pb: e.transpose(gp[pb][:, (j % 4) * 128:(j % 4 + 1) * 128], xn2[:, j * 128:(j + 1) * 128], ident[:]),
                     r=["xn2", "ident"], w=[GT[pb]])
            for j in range(8):
                pb = j // 4
                P.op('act', lambda e, j=j, pb=pb: e.activation(u2T[:, j, :], gp[pb][:, (j % 4) * 128:(j % 4 + 1) * 128], AF.Identity,
                                                             scale=cvec[:, V_W2, j:j + 1], bias=cvec[:, V_SH2, j:j + 1]),
                     r=[GT[pb], "cvec"], w=["u2T"])
            gpb2 = gp[2][:].bitcast(BF16)
            for j in range(8):
                P.op('pe', lambda e, j=j: e.transpose(gpb2[:, j * 128:(j + 1) * 128], u2T[:, j, :], identb[:]), r=["u2T", "identb"], w=[GT[2]])
            P.op('act', lambda e: e.copy(U2[:], gpb2), r=[GT[2]], w=[f"u2_{b}"])
            yield
            for g in range(16):
                pb = 2 + (g // 4) % 2
                for kc in range(8):
                    P.op('pe', lambda e, g=g, kc=kc, pb=pb: e.matmul(gp[pb][:, (g % 4) * 128:(g % 4 + 1) * 128], wqb[:, kc, g * 128:(g + 1) * 128],
                                                                   u2T[:, kc, :], start=(kc == 0), stop=(kc == 7)), r=["wqb", "u2T"], w=[GT[pb]])
                if g % 4 == 3:
                    P.op('act', lambda e, g=g, pb=pb: e.copy(qT[:, g - 3:g + 1, :].rearrange("p a b -> p (a b)"), gp[pb][:]), r=[GT[pb]], w=["qT"])
                    yield
            for g in range(16):
                pb = (g // 4) % 2
                P.op('pe', lambda e, g=g, pb=pb: e.matmul(gp[pb][:, (g % 4) * 128:(g % 4 + 1) * 128], qT[:, g, :], keysT[:, g, :],
                                                        start=True, stop=True), r=["qT", "keysT"], w=[GT[pb]])
                if g % 4 == 3:
                    P.op('act', lambda e, g=g, pb=pb: e.copy(s_[:, g - 3:g + 1, :].rearrange("p a b -> p (a b)"), gp[pb][:]), r=[GT[pb]], w=["s_"])
            yield
            for g in range(16):
                P.op('dve', lambda e, g=g: e.max(sc_[:, g, 0:8], s_[:, g, :]), r=["s_"], w=["sc_"])
                yield
                P.op('dve', lambda e, g=g: e.match_replace(s2_[:, g, :], sc_[:, g, 0:8], s_[:, g, :], NEG), r=["s_", "sc_"], w=["s2_"])
                yield
                P.op('dve', lambda e, g=g: e.max(sc_[:, g, 8:16], s2_[:, g, :]), r=["s2_"], w=["sc_"])
                yield
                P.op('dve', lambda e, g=g: e.max_index(idx_[:, g, 0:8], sc_[:, g, 0:8], s_[:, g, :]), r=["s_", "sc_"], w=["idx_"])
                yield
                P.op('dve', lambda e, g=g: e.max_index(idx_[:, g, 8:16], sc_[:, g, 8:16], s_[:, g, :]), r=["s_", "sc_"], w=["idx_"])
                yield
                if g % 2 == 1:
                    yield
            P.op('dve', lambda e: e.tensor_copy(idxf[:], idx_[:]), r=["idx_"], w=["idxf"])
            yield
            sc4 = sc_[:].rearrange("p (h two) k -> p h two k", two=2)
            ix4 = idxf[:].rearrange("p (h two) k -> p h two k", two=2)
            P.op('dve', lambda e: e.tensor_tensor(cand.rearrange("p h (a b) -> p h a b", b=16),
                                                  sc4[:, :, 0, :].unsqueeze(3).to_broadcast([128, 8, 16, 16]),
                                                  sc4[:, :, 1, :].unsqueeze(2).to_broadcast([128, 8, 16, 16]), ALU.add),
                 r=["sc_"], w=["s_"])
            yield
            yield
            for hh in range(8):
                P.op('dve', lambda e, hh=hh: e.max(top_[:, hh, 0:8], cand[:, hh, :]), r=["s_"], w=["top_"])
                yield
                P.op('dve', lambda e, hh=hh: e.match_replace(cand2[:, hh, :], top_[:, hh, 0:8], cand[:, hh, :], NEG), r=["s_", "top_"], w=["s2_"])
                yield
                P.op('dve', lambda e, hh=hh: e.max(top_[:, hh, 8:16], cand2[:, hh, :]), r=["s2_"], w=["top_"])
                yield
                P.op('dve', lambda e, hh=hh: e.max_index(pos_[:, hh, 0:8], top_[:, hh, 0:8], cand[:, hh, :]), r=["s_", "top_"], w=["pos_"])
                yield
                P.op('dve', lambda e, hh=hh: e.max_index(pos_[:, hh, 8:16], top_[:, hh, 8:16], cand[:, hh, :]), r=["s_", "top_"], w=["pos_"])
                yield
                yield
            P.op('dve', lambda e: e.tensor_scalar(pa_[:], pos_[:], 4, None, ALU.logical_shift_right), r=["pos_"], w=["pa_"])
            yield
            P.op('dve', lambda e: e.tensor_copy(paf[:], pa_[:]), r=["pa_"], w=["paf"])
            yield
            P.op('dve', lambda e: e.tensor_scalar(pa_[:], pos_[:], 15, None, ALU.bitwise_and), r=["pos_"], w=["pa_"])
            yield
            P.op('dve', lambda e: e.tensor_copy(pbf[:], pa_[:]), r=["pa_"], w=["pbf"])
            yield
            yield
            for which, (pf, dst) in enumerate(((paf, isel), (pbf, jsel))):
                P.op('dve', lambda e, pf=pf: e.tensor_tensor(oh, pf[:].unsqueeze(3).to_broadcast([128, 8, 16, 16]),
                                                             io16[:].unsqueeze(1).unsqueeze(1).to_broadcast([128, 8, 16, 16]), ALU.is_equal),
                     r=["paf", "pbf", "io16"], w=["s2_"])
                yield
                P.op('dve', lambda e, which=which: e.tensor_tensor(oh, oh, ix4[:, :, which, :].unsqueeze(2).to_broadcast([128, 8, 16, 16]), ALU.mult),
                     r=["s2_", "idxf"], w=["s2_"])
                yield
                P.op('dve', lambda e, dst=dst: e.tensor_reduce(dst[:], oh.rearrange("p h k a -> p (h k) a"), AX.X, ALU.add),
                     r=["s2_"], w=["isel", "jsel"])
                yield
                yield
            P.op('dve', lambda e: e.scalar_tensor_tensor(isel[:], isel[:], 128.0, jsel[:], ALU.mult, ALU.add), r=["isel", "jsel"], w=["isel"])
            yield
            P.op('dve', lambda e: e.tensor_copy(EIDX[:], isel[:]), r=["isel"], w=[f"eidx{b}"])
            yield
            P.op('dve', lambda e: e.tensor_tensor(g3, top_[:], top_[:, :, 0:1].to_broadcast([128, 8, 16]), ALU.subtract), r=["top_"], w=[f"gate{b}"])
            yield
            P.op('act', lambda e: e.activation(GATE[:], GATE[:], AF.Exp), r=[f"gate{b}"], w=[f"gate{b}"])
            P.op('dve', lambda e: e.tensor_reduce(gsum[:], g3, AX.X, ALU.add), r=[f"gate{b}"], w=["gsum"])
            yield
            P.op('dve', lambda e: e.reciprocal(gsum[:], gsum[:]), r=["gsum"], w=["gsum"])
            yield
            P.op('dve', lambda e: e.tensor_tensor(g3, g3, gsum[:].unsqueeze(2).to_broadcast([128, 8, 16]), ALU.mult), r=[f"gate{b}", "gsum"], w=[f"gate{b}"])
            yield
            yield

        def back(i, nxt):
            b = i % 2
            H1, U2, EIDX, GATE = h1[b], u2[b], eidx[b], gate[b]
            G4 = 4

            def slot_front(sl):
                k = sl % NGB
                P.dma('pool', lambda e: e.indirect_dma_start(out=GB[k][:], out_offset=None, in_=tabUV,
                                                             in_offset=bass.IndirectOffsetOnAxis(ap=EIDX[:, sl:sl + 1], axis=0)),
                      r=[f"eidx{b}"], w=[f"GB{k}"])
                if sl % 2 == 0:
                    P.op('dve', lambda e: e.scalar_tensor_tensor(jk[:], GB[k][:, 0:1024], 1.0, U2[:], ALU.mult, ALU.mult,
                                                                 accum_out=act_[:, sl:sl + 1]),
                         r=[f"GB{k}", f"u2_{b}"], w=["jk", f"act{sl}"])
                else:
                    pq = (sl // 2) % 2
                    P.op('dve', lambda e: e.tensor_tensor(prodb[pq][:], GB[k][:, 0:1024], U2[:], ALU.mult),
                         r=[f"GB{k}", f"u2_{b}"], w=[f"prodb{pq}"])
                    P.op('act', lambda e: e.activation(prodb[pq][:], prodb[pq][:], AF.Copy, accum_out=act_[:, sl:sl + 1]),
                         r=[f"prodb{pq}"], w=[f"prodb{pq}", f"act{sl}"])

            NDG = 4

            def st_gelu(sl):
                P.op('act', lambda e: e.activation(gl_[:, sl:sl + 1], act_[:, sl:sl + 1], AF.Gelu), r=[f"act{sl}"], w=[f"gl{sl}"])

            def st_gate(sl):
                P.op('dve', lambda e: e.tensor_tensor(gl_[:, sl:sl + 1], gl_[:, sl:sl + 1], GATE[:, sl:sl + 1], ALU.mult),
                     r=[f"gl{sl}", f"gate{b}"], w=[f"gl{sl}"])

            def st_diag(sl):
                k2 = sl % NDG
                P.op('act', lambda e: e.activation(dgs[k2][:], ident[:], AF.Copy, scale=gl_[:, sl:sl + 1]),
                     r=["ident", f"gl{sl}"], w=[f"dgs{k2}"])

            def st_mm(sl):
                k = sl % NGB
                k2 = sl % NDG
                for ng in range(2):
                    P.op('pe', lambda e, ng=ng: e.matmul(zp[1][:, ng * 512:(ng + 1) * 512], dgs[k2][:],
                                                       GB[k][:, 1024 + ng * 512:1024 + (ng + 1) * 512],
                                                       start=(sl == 0), stop=(sl == 127)),
                         r=[f"dgs{k2}", f"GB{k}"], w=[ZT[1][ng]])

            L1, L2, L3, L4 = 2, 4, 6, 8
            for p_ in range(128 + L4):
                if p_ < 128:
                    slot_front(p_)
                    if nxt is not None:
                        next(nxt, None)
                        if p_ % 2 == 1:
                            next(nxt, None)
                for L, fn in ((L1, st_gelu), (L2, st_gate), (L3, st_diag), (L4, st_mm)):
                    if 0 <= p_ - L < 128:
                        fn(p_ - L)
            if nxt is not None:
                for _ in nxt:
                    pass
            P.op('dve', lambda e: e.tensor_tensor(fin[:], zp[1][:], RG2, ALU.mult), r=ZT[1] + ["rowv"], w=["fin"])
            P.op('dve', lambda e: e.tensor_tensor(H1[:], H1[:], fin[:], ALU.add), r=[f"h1_{b}", "fin"], w=[f"h1_{b}"])
            P.op('dve', lambda e: e.scalar_tensor_tensor(fin[:], H1[:], 1.0, H1[:], ALU.mult, ALU.mult, accum_out=st3[:, 0:1]),
                 r=[f"h1_{b}"], w=["fin", "st3"])
            P.op('act', lambda e: e.activation(st3[:, 1:2], st3[:, 0:1], AF.Sqrt, scale=1.0 / D, bias=EPS), r=["st3"], w=["st3"])
            P.op('dve', lambda e: e.reciprocal(st3[:, 2:3], st3[:, 1:2]), r=["st3"], w=["st3"])
            P.op('dve', lambda e: e.scalar_tensor_tensor(fin[:], H1[:], st3[:, 2:3], RFG, ALU.mult, ALU.mult), r=[f"h1_{b}", "st3", "rowv"], w=["fin"])
            P.dma('sp', lambda e: e.dma_start(out=out[i * 128:(i + 1) * 128, :], in_=fin[:]), r=["fin"], w=["out"])

        for _ in front(0):
            pass
        for i in range(NT):
            back(i, front(i + 1) if i + 1 < NT else None)

        P.finish(["out"])
    return nc


_CACHE = {}


def kernel(**inputs):
    inp = {k: np.ascontiguousarray(np.asarray(v, dtype=np.float32)) for k, v in inputs.items()}
    if "nc" not in _CACHE:
        _CACHE["nc"] = build_nc()
    nc = _CACHE["nc"]
    shared = {
        "ada_w": inp["ada_w"][0], "ada_b": inp["ada_b"][0], "norm1_g": inp["norm1_g"][0], "w_in": inp["w_in"][0],
        "lb_logits": inp["lb_logits"], "hg_norm_g": inp["hg_norm_g"][0], "w_a": inp["w_a"][0],
        "conv_w": inp["conv_w"][0], "conv_b": inp["conv_b"][0], "conv_ln_g": inp["conv_ln_g"][0],
        "conv_ln_b": inp["conv_ln_b"][0], "w_b": inp["w_b"][0], "w_out": inp["w_out"][0],
        "norm2_g": inp["norm2_g"][0], "peer_wq": inp["peer_wq"][0],
        "peer_keys": inp["peer_keys"][0].reshape(16, 128, 128), "peer_u": inp["peer_u"][0],
        "peer_v": inp["peer_v"][0], "final_g": inp["final_g"],
    }
    in_maps = []
    for b in range(NCORES):
        m = dict(shared)
        m["x"] = inp["x"][b]
        m["c"] = inp["c"][b]
        in_maps.append(m)
    res = run_bass_kernel_spmd(nc, in_maps, core_ids=list(range(NCORES)))
    return np.stack([np.asarray(r["out"], dtype=np.float32) for r in res.results], axis=0)
```

```python
import numpy as np
from contextlib import ExitStack
import concourse.bass as bass
import concourse.mybir as mybir
from concourse.bass_utils import run_bass_kernel_spmd

F32 = mybir.dt.float32
BF16 = mybir.dt.bfloat16
U32 = mybir.dt.uint32
I32 = mybir.dt.int32
AF = mybir.ActivationFunctionType
ALU = mybir.AluOpType
AX = mybir.AxisListType

S = 2048
D = 1024
NT = 16
EPS = 1e-6
NCORES = 8


class Prog:
    def __init__(self, nc):
        self.nc = nc
        self.stack = ExitStack()
        self.E = dict(pe=nc.tensor, act=nc.scalar, dve=nc.vector, pool=nc.gpsimd, sp=nc.sync)
        self.sem = {k: self.stack.enter_context(nc.semaphore("sem_" + k)) for k in self.E}
        self.cnt = {k: 0 for k in self.E}
        self.known = {k: {} for k in self.E}
        self.NDS = 64
        self.dsem = [self.stack.enter_context(nc.semaphore(f"dsem{i}")) for i in range(self.NDS)]
        self.dcnt = [0] * self.NDS
        self.dnext = 0
        self.dnext_q = {}
        self.res = {}
        self.nins = 0

    def _wait(self, e, key, val):
        if val <= 0:
            return
        if e == 'pe' and key == 'pe':
            return
        if self.known[e].get(key, 0) >= val:
            return
        sem = self.sem[key] if isinstance(key, str) else self.dsem[key[1]]
        self.E[e].wait_ge(sem, val)
        self.known[e][key] = val

    def _deps(self, r, w):
        deps = {}

        def add(k, v):
            if deps.get(k, 0) < v:
                deps[k] = v
        for t in r:
            st = self.res.get(t)
            if st and st[0]:
                add(*st[0])
        for t in w:
            st = self.res.get(t)
            if st:
                if st[0]:
                    add(*st[0])
                for k, v in st[1].items():
                    add(k, v)
        return deps

    def _record(self, me, r, w):
        for t in r:
            st = self.res.setdefault(t, [None, {}])
            if st[1].get(me[0], 0) < me[1]:
                st[1][me[0]] = me[1]
        for t in w:
            self.res[t] = [me, {}]

    def op(self, e, fn, r=(), w=()):
        for k, v in self._deps(r, w).items():
            self._wait(e, k, v)
        ins = fn(self.E[e])
        self.cnt[e] += 1
        ins.then_inc(self.sem[e], 1)
        self._record((e, self.cnt[e]), r, w)
        self.nins += 1

    def dma(self, q, fn, r=(), w=()):
        for k, v in self._deps(r, w).items():
            self._wait(q, k, v)
        half = self.NDS // 2
        base = 0 if q == 'sp' else half
        i = base + self.dnext_q.get(q, 0)
        self.dnext_q[q] = (self.dnext_q.get(q, 0) + 1) % half
        self._wait(q, ('d', i), self.dcnt[i])
        ins = fn(self.E[q])
        self.dcnt[i] += 16
        ins.then_inc(self.dsem[i], 16)
        self._record((('d', i), self.dcnt[i]), r, w)
        self.nins += 1

    def barrier(self, skip_pool_dma=False):
        nd = self.NDS // 2 if skip_pool_dma else self.NDS
        for e in self.E:
            if skip_pool_dma and e == 'pool':
                continue
            for k in self.E:
                if k != e:
                    self._wait(e, k, self.cnt[k])
            for i in range(nd):
                self._wait(e, ('d', i), self.dcnt[i])

    def finish(self, tokens):
        for t in tokens:
            st = self.res.get(t)
            if st and st[0]:
                self._wait('sp', st[0][0], st[0][1])


def build_nc(stage=99, dbg_shape=None):
    nc = bass.Bass("TRN2", target_bir_lowering=False)
    dram = {}

    def din(name, shape):
        dram[name] = nc.dram_tensor(name, list(shape), F32, kind="ExternalInput").ap()
        return dram[name]
    x = din("x", [S, D])
    c = din("c", [D])
    ada_w = din("ada_w", [D, 6 * D])
    ada_b = din("ada_b", [6 * D])
    norm1_g = din("norm1_g", [D])
    w_in = din("w_in", [D, 8 * D])
    lb_logits = din("lb_logits", [2, D])
    hg_norm_g = din("hg_norm_g", [D])
    w_a = din("w_a", [D, D])
    conv_w = din("conv_w", [31, D])
    conv_b = din("conv_b", [D])
    conv_ln_g = din("conv_ln_g", [D])
    conv_ln_b = din("conv_ln_b", [D])
    w_b = din("w_b", [D, D])
    w_out = din("w_out", [D, D])
    norm2_g = din("norm2_g", [D])
    peer_wq = din("peer_wq", [D, 2 * D])
    peer_keys = din("peer_keys", [16, 128, 128])
    peer_u = din("peer_u", [16384, D])
    peer_v = din("peer_v", [16384, D])
    final_g = din("final_g", [D])
    out = nc.dram_tensor("out", [S, D], F32, kind="ExternalOutput").ap()
    tabUV = nc.dram_tensor("tabUV", [16384, 2048], BF16, kind="Internal").ap()
    h1s = nc.dram_tensor("h1s", [S, D], F32, kind="Internal").ap()
    dbg = None
    if dbg_shape is not None:
        dbg = nc.dram_tensor("dbg", list(dbg_shape), F32, kind="ExternalOutput").ap()

    P = Prog(nc)
    ES = ExitStack()

    def sb(name, shape, dt=F32, stack=None):
        return (stack or ES).enter_context(nc.sbuf_tensor(name, list(shape), dt))

    def ps(name, shape, dt=F32):
        return ES.enter_context(nc.psum_tensor(name, list(shape), dt))

    with P.stack, ES:
        zp = [ps("zp0", [128, 1024]), ps("zp1", [128, 1024])]
        gp = [ps(f"gp{i}", [128, 512]) for i in range(4)]
        ZT = [["zp0a", "zp0b"], ["zp1a", "zp1b"]]
        GT = ["gp0", "gp1", "gp2", "gp3"]

        ident = sb("ident", [128, 128], F32)
        identb = sb("identb", [128, 128], BF16)
        ones_b = sb("ones_b", [128, 128], BF16)
        onesD_b = sb("onesD_b", [128, 128], BF16)
        ones_f = sb("ones_f", [128, 128], F32)
        iot_i = sb("iot_i", [128, 128], I32)
        iop_i = sb("iop_i", [128, 128], I32)
        P.op('pool', lambda e: e.iota(iot_i[:], [[1, 128]], base=0, channel_multiplier=0), w=["iot_i"])
        P.op('pool', lambda e: e.iota(iop_i[:], [[0, 128]], base=0, channel_multiplier=1), w=["iop_i"])
        P.op('dve', lambda e: e.tensor_tensor(ident[:], iot_i[:], iop_i[:], ALU.is_equal), r=["iot_i", "iop_i"], w=["ident"])
        P.op('dve', lambda e: e.tensor_copy(identb[:], ident[:]), r=["ident"], w=["identb"])
        P.op('dve', lambda e: e.memset(ones_b[:], 1.0 / 128), w=["ones_b"])
        P.op('dve', lambda e: e.memset(onesD_b[:], 1.0 / 1024), w=["onesD_b"])
        P.op('dve', lambda e: e.memset(ones_f[:], 1.0), w=["ones_f"])

        stg = sb("stg", [128, 128], F32)
        rows = [(c, 8), (ada_b, 48), (norm1_g, 8), (lb_logits[0, :], 8), (lb_logits[1, :], 8), (hg_norm_g, 8),
                (conv_b, 8), (conv_ln_g, 8), (conv_ln_b, 8), (norm2_g, 8), (final_g, 8)]
        r0 = 0
        offs = []
        for apx, n in rows:
            offs.append(r0)
            P.dma('sp', lambda e, apx=apx, r0=r0, n=n: e.dma_start(out=stg[r0:r0 + n, :], in_=apx.rearrange("(j p) -> j p", p=128)),
                  w=["stg"])
            r0 += n
        assert r0 == 128
        (O_C, O_ADAB, O_N1G, O_LB0, O_LB1, O_HGG, O_CB, O_LNG, O_LNB, O_N2G, O_FG) = offs
        colp = sb("colp", [128, 128], F32)
        P.op('pe', lambda e: e.transpose(gp[0][:, 0:128], stg[:], ident[:]), r=["stg", "ident"], w=[GT[0]])
        P.op('dve', lambda e: e.tensor_copy(colp[:], gp[0][:, 0:128]), r=[GT[0]], w=["colp"])
        cw = sb("cw", [128, 256], F32)
        stg2 = sb("stg2", [128, 2, 128], F32)
        P.op('dve', lambda e: e.memset(stg2[:], 0.0), w=["stg2"])
        cwv = conv_w.rearrange("k (j p) -> (k j) p", p=128)
        P.dma('sp', lambda e: e.dma_start(out=stg2[:, 0, :], in_=cwv[0:128, :]), w=["stg2"])
        P.dma('sp', lambda e: e.dma_start(out=stg2[0:120, 1, :], in_=cwv[128:248, :]), w=["stg2"])
        for hh in range(2):
            P.op('pe', lambda e, hh=hh: e.transpose(gp[1][:, hh * 128:(hh + 1) * 128], stg2[:, hh, :], ident[:]),
                 r=["stg2", "ident"], w=[GT[1]])
        P.op('dve', lambda e: e.tensor_copy(cw[:], gp[1][:, 0:256]), r=[GT[1]], w=["cw"])

        sc_col = sb("sc_col", [128, 8], F32)
        ada_col = sb("ada_col", [128, 48], F32)
        NV = 12
        cvec = sb("cvec", [128, NV, 8], F32)
        phM = ExitStack()
        uT = sb("uT", [128, 8, S], BF16, phM)
        wg = [sb(f"wg{i}", [128, 8, 128], BF16, phM) for i in range(4)]
        wgf = [sb(f"wgf{i}", [128, 8, 128], F32, phM) for i in range(1)]
        oaT = sb("oaT", [128, 8, S], BF16, phM)
        NCB = 8
        cbuf = [oaT[:, i, :].rearrange("p (r d) -> p r d", r=2) for i in range(NCB)]
        tU = peer_u.rearrange("(c p r) d -> c p r d", p=128, r=2)
        tV = peer_v.rearrange("(c p r) d -> c p r d", p=128, r=2)
        tO = tabUV.rearrange("(c p r) d -> c p r d", p=128, r=2)
        jobs = [(tU, c, 0) for c in range(64)] + [(tV, c, 1024) for c in range(64)]

        def conv_store(n):
            tv_, c_, off_ = jobs[n]
            P.dma('pool', lambda e: e.dma_start(out=tO[c_][:, :, off_:off_ + 1024], in_=cbuf[n % NCB]), r=[f"cbuf{n % NCB}"], w=["tabUV"])
        for n, (tv_, c_, off_) in enumerate(jobs):
            P.dma('pool', lambda e, tv_=tv_, c_=c_, n=n: e.dma_start(out=cbuf[n % NCB], in_=tv_[c_]), w=[f"cbuf{n % NCB}"])
            if n >= 4:
                conv_store(n - 4)
        for n in range(len(jobs) - 4, len(jobs)):
            conv_store(n)

        P.op('act', lambda e: e.activation(sc_col[:], colp[:, O_C:O_C + 8], AF.Silu), r=["colp"], w=["sc_col"])

        with ExitStack() as st_ada:
            awt = [sb(f"awt{i}", [128, 8, 512], F32, st_ada) for i in range(2)]
            awv = ada_w.rearrange("(kc p) n -> p kc n", p=128)
            for g4 in range(12):
                bi = g4 % 2
                P.dma('sp', lambda e, g4=g4, bi=bi: e.dma_start(out=awt[bi][:], in_=awv[:, :, g4 * 512:(g4 + 1) * 512]),
                      w=[f"awt{bi}"])
                for gg in range(4):
                    col = g4 * 4 + gg
                    for kc in range(8):
                        P.op('pe', lambda e, bi=bi, gg=gg, kc=kc, col=col: e.matmul(
                            gp[2][:, col:col + 1], awt[bi][:, kc, gg * 128:(gg + 1) * 128], sc_col[:, kc:kc + 1],
                            start=(kc == 0), stop=(kc == 7)), r=[f"awt{bi}", "sc_col"], w=[GT[2]])
            P.op('dve', lambda e: e.tensor_tensor(ada_col[:], gp[2][:, 0:48], colp[:, O_ADAB:O_ADAB + 48], ALU.add),
                 r=[GT[2], "colp"], w=["ada_col"])
        P.barrier(skip_pool_dma=True)
        (V_W1, V_SH1, V_W2, V_SH2, V_G1, V_G2, V_LB, V_OML, V_NOML, V_FG, V_TMP, V_TMP2) = range(NV)
        A_SH1, A_SC1, A_G1, A_SH2, A_SC2, A_G2 = [ada_col[:, i * 8:(i + 1) * 8] for i in range(6)]
        R_ = ["ada_col", "colp", "cvec"]
        P.op('dve', lambda e: e.scalar_tensor_tensor(cvec[:, V_W1, :], A_SC1, 1.0, colp[:, O_N1G:O_N1G + 8], ALU.add, ALU.mult), r=R_, w=["cvec"])
        P.op('dve', lambda e: e.tensor_copy(cvec[:, V_SH1, :], A_SH1), r=R_, w=["cvec"])
        P.op('dve', lambda e: e.scalar_tensor_tensor(cvec[:, V_W2, :], A_SC2, 1.0, colp[:, O_N2G:O_N2G + 8], ALU.add, ALU.mult), r=R_, w=["cvec"])
        P.op('dve', lambda e: e.tensor_copy(cvec[:, V_SH2, :], A_SH2), r=R_, w=["cvec"])
        P.op('dve', lambda e: e.tensor_copy(cvec[:, V_G1, :], A_G1), r=R_, w=["cvec"])
        P.op('dve', lambda e: e.tensor_copy(cvec[:, V_G2, :], A_G2), r=R_, w=["cvec"])
        P.op('dve', lambda e: e.tensor_copy(cvec[:, V_FG, :], colp[:, O_FG:O_FG + 8]), r=R_, w=["cvec"])
        P.op('dve', lambda e: e.tensor_tensor(cvec[:, V_TMP, :], colp[:, O_LB0:O_LB0 + 8], colp[:, O_LB1:O_LB1 + 8], ALU.subtract), r=R_, w=["cvec"])
        P.op('act', lambda e: e.activation(cvec[:, V_LB, :], cvec[:, V_TMP, :], AF.Sigmoid), r=["cvec"], w=["cvec"])
        P.op('dve', lambda e: e.tensor_scalar(cvec[:, V_OML, :], cvec[:, V_LB, :], -1.0, 1.0, ALU.mult, ALU.add), r=["cvec"], w=["cvec"])
        P.op('dve', lambda e: e.tensor_scalar(cvec[:, V_NOML, :], cvec[:, V_OML, :], -1.0, None, ALU.mult), r=["cvec"], w=["cvec"])

        if stage == 0:
            P.dma('sp', lambda e: e.dma_start(out=dbg[0:128, 0:NV * 8], in_=cvec[:].rearrange("p a b -> p (a b)")), r=["cvec"], w=["dbg"])
            P.dma('sp', lambda e: e.dma_start(out=dbg[128:256, :], in_=rowv[:, 0, :]), r=["rowv"], w=["dbg"])
            P.dma('sp', lambda e: e.dma_start(out=dbg[256:384, 0:256], in_=cw[:]), r=["cw"], w=["dbg"])
            P.dma('sp', lambda e: e.dma_start(out=dbg[384:512, 0:48], in_=ada_col[:]), r=["ada_col"], w=["dbg"])
            P.finish(["dbg"])
            return nc


        ph1 = ExitStack()
        xt = [sb(f"xt{i}", [128, 1024], F32, ph1) for i in range(2)]
        xn = [sb(f"xn{i}", [128, 1024], F32, ph1) for i in range(2)]
        junk = sb("junk", [128, 1024], F32, ph1)
        st1 = sb("st1", [128, 2, 4], F32, ph1)
        for i in range(NT):
            b = i % 2
            P.dma('sp', lambda e, i=i, b=b: e.dma_start(out=xt[b][:], in_=x[i * 128:(i + 1) * 128, :]), w=[f"xt{b}"])
            P.op('dve', lambda e, b=b: e.scalar_tensor_tensor(junk[:], xt[b][:], 1.0, xt[b][:], ALU.mult, ALU.mult,
                                                              accum_out=st1[:, b, 0:1]), r=[f"xt{b}"], w=["junk", f"st1{b}"])
            P.op('act', lambda e, b=b: e.activation(st1[:, b, 1:2], st1[:, b, 0:1], AF.Sqrt, scale=1.0 / D, bias=EPS),
                 r=[f"st1{b}"], w=[f"st1{b}"])
            P.op('dve', lambda e, b=b: e.reciprocal(st1[:, b, 2:3], st1[:, b, 1:2]), r=[f"st1{b}"], w=[f"st1{b}"])
            P.op('dve', lambda e, b=b: e.tensor_scalar(xn[b][:], xt[b][:], st1[:, b, 2:3], None, ALU.mult),
                 r=[f"xt{b}", f"st1{b}"], w=[f"xn{b}"])
            for j in range(8):
                pb = j // 4
                P.op('pe', lambda e, b=b, j=j, pb=pb: e.transpose(gp[pb][:, (j % 4) * 128:(j % 4 + 1) * 128],
                                                               xn[b][:, j * 128:(j + 1) * 128], ident[:]),
                     r=[f"xn{b}", "ident"], w=[GT[pb]])
            for j in range(8):
                pb = j // 4
                P.op('act', lambda e, i=i, j=j, pb=pb: e.activation(
                    uT[:, j, i * 128:(i + 1) * 128], gp[pb][:, (j % 4) * 128:(j % 4 + 1) * 128], AF.Identity,
                    scale=cvec[:, V_W1, j:j + 1], bias=cvec[:, V_SH1, j:j + 1]), r=[GT[pb], "cvec"], w=["uT"])
        ph1.close()
        P.barrier(skip_pool_dma=True)

        NWG = 4
        wctr = [0]

        def load_wg(W, col0):
            b = wctr[0] % NWG
            bf = 0
            wctr[0] += 1
            Wv = W.rearrange("(kc p) n -> p kc n", p=128)
            P.dma('sp', lambda e: e.dma_start(out=wgf[bf][:], in_=Wv[:, :, col0:col0 + 128]), w=[f"wgf{bf}"])
            P.op('act', lambda e: e.copy(wg[b][:], wgf[bf][:]), r=[f"wgf{bf}"], w=[f"wg{b}"])
            return wg[b], f"wg{b}"

        zctr = [0]

        def zmm(wt, wtok, half):
            b = zctr[0] % 2
            zctr[0] += 1
            for tg in range(2):
                t0 = half * 1024 + tg * 512
                for kc in range(8):
                    P.op('pe', lambda e, tg=tg, kc=kc, t0=t0: e.matmul(zp[b][:, tg * 512:(tg + 1) * 512], wt[:, kc, :],
                                                                    uT[:, kc, t0:t0 + 512], start=(kc == 0), stop=(kc == 7)),
                         r=[wtok, "uT"], w=[ZT[b][tg]])
            return b

        if stage == 1:
            dbt = sb("dbt", [128, 1024], F32)
            for j in range(8):
                P.op('dve', lambda e, j=j: e.tensor_copy(dbt[:], uT[:, j, 0:1024]), r=["uT"], w=["dbt"])
                P.dma('sp', lambda e, j=j: e.dma_start(out=dbg[j * 128:(j + 1) * 128, :], in_=dbt[:]), r=["dbt"], w=["dbg"])
            P.finish(["dbg"])
            return nc

        cvT = sb("cvT", [128, 8, S], BF16, phM)
        phB = ExitStack()
        glu = sb("glu", [128, 8, 32 + S], BF16, phB)
        PADL = 32
        dgc = [sb(f"dgc{i}", [128, 31, 128], BF16, phB) for i in range(2)]
        sgb = [sb(f"sgb{i}", [128, 1024], F32, phB) for i in range(2)]
        sqt = [sb(f"sqt{i}", [128, 512], BF16, phB) for i in range(2)]
        lnt = [sb(f"lnt{i}", [128, 512], F32, phB) for i in range(4)]
        for j in range(8):
            P.op('dve', lambda e, j=j: e.memset(glu[:, j, 0:PADL], 0.0), w=[f"glu{j}"])
        def b_loads(j):
            return load_wg(w_in, (32 + j) * 128), load_wg(w_in, (40 + j) * 128)

        def b_diag(j):
            dj = dgc[j % 2]
            for kk in range(31):
                P.op('dve', lambda e, kk=kk, dj=dj: e.tensor_scalar(dj[:, kk, :], ident[:], cw[:, kk * 8 + j:kk * 8 + j + 1], None, ALU.mult),
                     r=["ident", "cw"], w=[f"dgc{j % 2}"])

        wnext = b_loads(0)
        b_diag(0)
        for j in range(8):
            (wa_, ta_), (wb_, tb_) = wnext
            if j + 1 < 8:
                wnext = b_loads(j + 1)
            for half in range(2):
                bb = zmm(wb_, tb_, half)
                sb_ = sgb[half]
                P.op('act', lambda e, bb=bb, sb_=sb_: e.activation(sb_[:], zp[bb][:], AF.Sigmoid), r=ZT[bb], w=[f"sgb{half}"])
                ba = zmm(wa_, ta_, half)
                P.op('dve', lambda e, ba=ba, sb_=sb_, j=j, half=half: e.tensor_tensor(
                    glu[:, j, PADL + half * 1024:PADL + (half + 1) * 1024], zp[ba][:], sb_[:], ALU.mult),
                    r=ZT[ba] + [f"sgb{half}"], w=[f"glu{j}"])
            if j + 1 < 8:
                b_diag(j + 1)
            dj = dgc[j % 2]
            for tg in range(4):
                t0 = tg * 512
                pb = tg % 2
                for kk in range(31):
                    P.op('pe', lambda e, j=j, kk=kk, pb=pb, t0=t0, dj=dj: e.matmul(
                        gp[pb][:], dj[:, kk, :], glu[:, j, PADL - 30 + t0 + kk:PADL - 30 + t0 + kk + 512],
                        start=(kk == 0), stop=(kk == 30)), r=[f"dgc{j % 2}", f"glu{j}"], w=[GT[pb]])
                P.op('act', lambda e, j=j, pb=pb, t0=t0: e.activation(cvT[:, j, t0:t0 + 512], gp[pb][:], AF.Identity,
                                                                    bias=colp[:, O_CB + j:O_CB + j + 1]), r=[GT[pb], "colp"], w=[f"cvT{tg}"])
        if stage == 21:
            dbt = sb("dbt", [128, 1024], F32, phB)
            for j in range(8):
                P.op('dve', lambda e, j=j: e.tensor_copy(dbt[:], glu[:, j, PADL:PADL + 1024]), r=[f"glu{j}"], w=["dbt"])
                P.dma('sp', lambda e, j=j: e.dma_start(out=dbg[j * 128:(j + 1) * 128, :], in_=dbt[:]), r=["dbt"], w=["dbg"])
            for j in range(8):
                P.op('dve', lambda e, j=j: e.tensor_copy(dbt[:], cvT[:, j, 0:1024]), r=["cvT0", "cvT1"], w=["dbt"])
                P.dma('sp', lambda e, j=j: e.dma_start(out=dbg[1024 + j * 128:1024 + (j + 1) * 128, :], in_=dbt[:]), r=["dbt"], w=["dbg"])
            P.finish(["dbg"])
            phB.close()
            return nc
        for tg in range(4):
            t0 = tg * 512
            for j in range(8):
                P.op('pe', lambda e, j=j, t0=t0: e.matmul(gp[2][:], onesD_b[:], cvT[:, j, t0:t0 + 512], start=(j == 0), stop=(j == 7)),
                     r=["onesD_b", f"cvT{tg}"], w=[GT[2]])
            for j in range(8):
                P.op('act', lambda e, j=j, t0=t0: e.activation(sqt[j % 2][:], cvT[:, j, t0:t0 + 512], AF.Square), r=[f"cvT{tg}"], w=[f"sqt{j % 2}"])
                P.op('pe', lambda e, j=j, t0=t0: e.matmul(gp[3][:], onesD_b[:], sqt[j % 2][:], start=(j == 0), stop=(j == 7)),
                     r=["onesD_b", f"sqt{j % 2}"], w=[GT[3]])
            mean_, var_, rstd_, tmp_ = lnt
            P.op('act', lambda e: e.copy(mean_[:], gp[2][:]), r=[GT[2]], w=["lnt0"])
            P.op('dve', lambda e: e.tensor_tensor(var_[:], mean_[:], mean_[:], ALU.mult), r=["lnt0"], w=["lnt1"])
            P.op('dve', lambda e: e.tensor_tensor(var_[:], gp[3][:], var_[:], ALU.subtract), r=[GT[3], "lnt1"], w=["lnt1"])
            P.op('dve', lambda e: e.tensor_scalar(var_[:], var_[:], 0.0, None, ALU.max), r=["lnt1"], w=["lnt1"])
            P.op('act', lambda e: e.activation(rstd_[:], var_[:], AF.Sqrt, bias=EPS), r=["lnt1"], w=["lnt2"])
            P.op('dve', lambda e: e.reciprocal(rstd_[:], rstd_[:]), r=["lnt2"], w=["lnt2"])
            for j in range(8):
                P.op('dve', lambda e, j=j, t0=t0: e.tensor_tensor(tmp_[:], cvT[:, j, t0:t0 + 512], mean_[:], ALU.subtract),
                     r=[f"cvT{tg}", "lnt0"], w=["lnt3"])
                P.op('dve', lambda e: e.tensor_tensor(tmp_[:], tmp_[:], rstd_[:], ALU.mult), r=["lnt3", "lnt2"], w=["lnt3"])
                P.op('act', lambda e, j=j, t0=t0: e.activation(cvT[:, j, t0:t0 + 512], tmp_[:], AF.Silu,
                                                             scale=colp[:, O_LNG + j:O_LNG + j + 1], bias=colp[:, O_LNB + j:O_LNB + j + 1]),
                     r=["lnt3", "colp"], w=[f"cvT{tg}"])
        phB.close()
        P.barrier()

        if stage == 2:
            dbt = sb("dbt", [128, 1024], F32)
            for j in range(8):
                P.op('dve', lambda e, j=j: e.tensor_copy(dbt[:], cvT[:, j, 0:1024]), r=["cvT0","cvT1","cvT2","cvT3"], w=["dbt"])
                P.dma('sp', lambda e, j=j: e.dma_start(out=dbg[j * 128:(j + 1) * 128, :], in_=dbt[:]), r=["dbt"], w=["dbg"])
            for j in range(8):
                P.op('dve', lambda e, j=j: e.tensor_copy(dbt[:], cvT[:, j, 1024:2048]), r=["cvT0","cvT1","cvT2","cvT3"], w=["dbt"])
                P.dma('sp', lambda e, j=j: e.dma_start(out=dbg[1024 + j * 128:1024 + (j + 1) * 128, :], in_=dbt[:]), r=["dbt"], w=["dbg"])
            P.finish(["dbg"])
            return nc


        phA = ExitStack()
        hA = sb("hA", [128, S], F32, phA)
        hB = sb("hB", [128, S], F32, phA)
        hC = [sb(f"hC{i}", [128, S], F32, phA) for i in range(2)]
        msk = sb("msk", [128, S], BF16, phA)
        qt = [sb(f"qt{i}", [128, S], BF16, phA) for i in range(2)]
        kt = [sb(f"kt{i}", [128, S], BF16, phA) for i in range(2)]
        vT = [sb(f"vT{i}", [128, S], BF16, phA) for i in range(2)]
        sg = [sb(f"sg{i}", [128, S], BF16, phA) for i in range(2)]
        vtok2 = [sb(f"vtok{i}", [32, 8, 128], BF16, phA) for i in range(2)]
        ktok = sb("ktok", [32, 8, 128], BF16, phA)
        Sall = sb("Sall", [128, 9, 128], BF16, phA)
        Rst2 = [sb(f"Rst{i}", [128, 128], F32, phA) for i in range(2)]
        nhalf = sb("nhalf", [128, 256], F32, phA)
        msb = sb("msb", [128, 256], F32, phA)
        PT2 = [sb(f"PT{i}", [32, 8, 32], BF16, phA) for i in range(2)]
        tri = sb("tri", [32, 32], F32, phA)
        osq = sb("osq", [128, 256], BF16, phA)
        lsc = sb("lsc", [128, 3, 256], F32, phA)
        rs_, tmpo, hOq = lsc[:, 0, :], lsc[:, 1, :], lsc[:, 2, :]
        hOi = hA[:].bitcast(I32)
        P.op('pool', lambda e: e.iota(hOi, [[1, S]], base=0, channel_multiplier=0), w=["hA"])
        P.op('dve', lambda e: e.tensor_scalar(hOi, hOi, 31, None, ALU.bitwise_and), r=["hA"], w=["hA"])
        P.op('dve', lambda e: e.tensor_scalar(msk[:], hOi, 0.0, None, ALU.is_gt), r=["hA"], w=["msk"])
        P.op('dve', lambda e: e.tensor_tensor(tri[:], iot_i[0:32, 0:32], iop_i[0:32, 0:32], ALU.is_ge), r=["iot_i", "iop_i"], w=["tri"])
        gpb = [g_[:].bitcast(BF16) for g_ in gp]
        P.op('dve', lambda e: e.memset(nhalf[:], -0.5), w=["nhalf"])

        def zmm0(wt, wtok, half):
            for tg in range(2):
                t0 = half * 1024 + tg * 512
                for kc in range(8):
                    P.op('pe', lambda e, tg=tg, kc=kc, t0=t0: e.matmul(zp[0][:, tg * 512:(tg + 1) * 512], wt[:, kc, :],
                                                                    uT[:, kc, t0:t0 + 512], start=(kc == 0), stop=(kc == 7)),
                         r=[wtok, "uT"], w=[ZT[0][tg]])

        def prologue(h):
            p = h % 2
            HC, QT, KT, VT, SG = hC[p], qt[p], kt[p], vT[p], sg[p]
            wf_, tf_ = load_wg(w_in, (8 + h) * 128)
            wq_, tq_ = load_wg(w_in, h * 128)
            wi_, ti_ = load_wg(w_in, (16 + h) * 128)
            wg_, tg_ = load_wg(w_in, (24 + h) * 128)
            for half in range(2):
                zmm0(wf_, tf_, half)
                P.op('act', lambda e, half=half: e.activation(hA[:, half * 1024:(half + 1) * 1024], zp[0][:], AF.Sigmoid),
                     r=ZT[0], w=["hA"])
                yield
            P.op('act', lambda e: e.activation(hB[:], hA[:], AF.Ln, scale=cvec[:, V_OML, h:h + 1], bias=cvec[:, V_LB, h:h + 1]),
                 r=["hA", "cvec"], w=["hB"])
            P.op('dve', lambda e: e.tensor_scalar(hA[:], hA[:], cvec[:, V_NOML, h:h + 1], cvec[:, V_OML, h:h + 1], ALU.mult, ALU.add),
                 r=["hA", "cvec"], w=["hA"])
            P.op('dve', lambda e: e.tensor_tensor_scan(HC[:], msk[:], hB[:], 0.0, ALU.mult, ALU.add), r=["msk", "hB"], w=[f"hC{p}"])
            P.op('act', lambda e: e.activation(hB[:], HC[:], AF.Exp, scale=-1.0), r=[f"hC{p}"], w=["hB"])
            P.op('dve', lambda e: e.tensor_tensor(KT[:], hA[:], hB[:], ALU.mult), r=["hA", "hB"], w=[f"kt{p}"])
            P.op('act', lambda e: e.activation(HC[:], HC[:], AF.Exp), r=[f"hC{p}", "hB"], w=[f"hC{p}"])
            for half in range(2):
                zmm0(wq_, tq_, half)
                P.op('dve', lambda e, half=half: e.tensor_tensor(QT[:, half * 1024:(half + 1) * 1024], zp[0][:],
                                                               HC[:, half * 1024:(half + 1) * 1024], ALU.mult),
                     r=ZT[0] + [f"hC{p}"], w=[f"qt{p}"])
                yield
            for half in range(2):
                zmm0(wi_, ti_, half)
                P.op('act', lambda e, half=half: e.copy(VT[:, half * 1024:(half + 1) * 1024], zp[0][:]), r=ZT[0], w=[f"vT{p}"])
                yield
            for half in range(2):
                zmm0(wg_, tg_, half)
                P.op('act', lambda e, half=half: e.activation(SG[:, half * 1024:(half + 1) * 1024], zp[0][:], AF.Silu),
                     r=ZT[0], w=[f"sg{p}"])
                yield

        def chunk_loop(h, nxt):
            p = h % 2
            HC, QT, KT, VT, SG = hC[p], qt[p], kt[p], vT[p], sg[p]
            tHC, tQT, tKT, tVT, tSG = f"hC{p}", f"qt{p}", f"kt{p}", f"vT{p}", f"sg{p}"
            P.op('dve', lambda e: e.memset(Sall[:, 0, :], 0.0), w=["Sall"])

            def g_T_SC(qd):
                c0 = qd * 8
                for cc in range(8):
                    t0 = (c0 + cc) * 32
                    P.op('pe', lambda e, cc=cc, t0=t0: e.transpose(gpb[0][0:32, cc * 128:(cc + 1) * 128],
                                                                 KT[:, t0:t0 + 32], identb[:]), r=[tKT, "identb"], w=[GT[0]])
                    P.op('pe', lambda e, cc=cc, t0=t0: e.transpose(gpb[2][0:32, cc * 128:(cc + 1) * 128],
                                                                 VT[:, t0:t0 + 32], identb[:]), r=[tVT, "identb"], w=[GT[2]])
                for cc in range(8):
                    t0 = (c0 + cc) * 32
                    P.op('pe', lambda e, cc=cc, t0=t0: e.matmul(zp[1][0:32, cc * 32:(cc + 1) * 32], KT[:, t0:t0 + 32], QT[:, t0:t0 + 32],
                                                              start=True, stop=True), r=[tKT, tQT], w=[ZT[1][0]])

            def g_evac_mask(qd):
                pv = qd % 2
                P.op('act', lambda e: e.copy(ktok[:].rearrange("p a b -> p (a b)"), gpb[0][0:32, :]), r=[GT[0]], w=["ktok"])
                P.op('dve', lambda e: e.tensor_copy(vtok2[pv][:].rearrange("p a b -> p (a b)"), gpb[2][0:32, :]), r=[GT[2]], w=[f"vtok{pv}"])
                P.op('dve', lambda e: e.tensor_tensor(PT2[pv][:], zp[1][0:32, 0:256].rearrange("p (c t) -> p c t", t=32),
                                                      tri[:].unsqueeze(1).to_broadcast([32, 8, 32]), ALU.mult),
                     r=[ZT[1][0], "tri"], w=[f"PT{pv}"])

            def norm_tail(qd):
                q0 = qd * 256
                P.op('pe', lambda e: e.matmul(zp[1][:, 768:1024], ones_b[:], osq[:], start=True, stop=True), r=["ones_b", "osq"], w=[ZT[1][1]])
                P.op('act', lambda e: e.activation(rs_, zp[1][:, 768:1024], AF.Sqrt, bias=EPS), r=[ZT[1][1]], w=["rs_"])
                P.op('dve', lambda e: e.reciprocal(rs_, rs_), r=["rs_"], w=["rs_"])
                P.op('dve', lambda e: e.tensor_tensor(tmpo, hOq, rs_, ALU.mult), r=["hOq", "rs_"], w=["tmpo"])
                P.op('dve', lambda e: e.scalar_tensor_tensor(oaT[:, h, q0:q0 + 256], tmpo, colp[:, O_HGG + h:O_HGG + h + 1],
                                                             SG[:, q0:q0 + 256], ALU.mult, ALU.mult),
                     r=["tmpo", "colp", tSG], w=["oaT"])

            g_T_SC(0)
            g_evac_mask(0)
            for qd in range(8):
                c0 = qd * 8
                pv = qd % 2
                VTK, PTK = vtok2[pv], PT2[pv]
                for cc in range(8):
                    P.op('pe', lambda e, cc=cc: e.matmul(gp[1 + 2 * (cc // 4)][:, (cc % 4) * 128:(cc % 4 + 1) * 128], ktok[:, cc, :], VTK[:, cc, :],
                                                       start=True, stop=True), r=["ktok", f"vtok{pv}"], w=[GT[1 + 2 * (cc // 4)]])
                if qd < 7:
                    g_T_SC(qd + 1)
                for cc in range(8):
                    c = c0 + cc
                    kvap = gp[1 + 2 * (cc // 4)][:, (cc % 4) * 128:(cc % 4 + 1) * 128]
                    Rc, Rp = Rst2[c % 2], Rst2[(c + 1) % 2]
                    if c == 0:
                        P.op('dve', lambda e, kvap=kvap, Rc=Rc: e.tensor_copy(Rc[:], kvap), r=[GT[1 + 2 * (cc // 4)]], w=[f"Rst{c % 2}"])
                    else:
                        dprev = HC[:, 32 * (c - 1) + 31:32 * (c - 1) + 32]
                        P.op('dve', lambda e, kvap=kvap, dprev=dprev, Rc=Rc, Rp=Rp: e.scalar_tensor_tensor(Rc[:], Rp[:], dprev, kvap, ALU.mult, ALU.add),
                             r=[GT[1 + 2 * (cc // 4)], f"Rst{(c + 1) % 2}", tHC], w=[f"Rst{c % 2}"])
                    dcur = HC[:, 32 * c + 31:32 * c + 32]
                    P.op('act', lambda e, cc=cc, dcur=dcur, Rc=Rc: e.activation(Sall[:, cc + 1, :], Rc[:], AF.Identity, scale=dcur),
                         r=[f"Rst{c % 2}", tHC], w=["Sall"])
                if qd < 7:
                    g_evac_mask(qd + 1)
                if qd >= 1:
                    norm_tail(qd - 1)
                if nxt is not None:
                    next(nxt, None)
                for cc in range(8):
                    t0 = (c0 + cc) * 32
                    P.op('pe', lambda e, cc=cc: e.matmul(zp[1][:, 512 + cc * 32:512 + (cc + 1) * 32], VTK[:, cc, :], PTK[:, cc, :],
                                                       start=True, stop=False), r=[f"vtok{pv}", f"PT{pv}"], w=[ZT[1][1]])
                    P.op('pe', lambda e, cc=cc, t0=t0: e.matmul(zp[1][:, 512 + cc * 32:512 + (cc + 1) * 32], Sall[:, cc, :], QT[:, t0:t0 + 32],
                                                              start=False, stop=True), r=["Sall", tQT], w=[ZT[1][1]])
                if qd < 7:
                    P.op('dve', lambda e: e.tensor_copy(Sall[:, 0, :], Sall[:, 8, :]), r=["Sall"], w=["Sall"])
                P.op('act', lambda e: e.copy(hOq, zp[1][:, 512:768]), r=[ZT[1][1]], w=["hOq"])
                P.op('act', lambda e: e.activation(osq[:], zp[1][:, 512:768], AF.Square), r=[ZT[1][1]], w=["osq"])
            norm_tail(7)
            if nxt is not None:
                for _ in nxt:
                    pass

        for _ in prologue(0):
            pass
        for h in range(8):
            chunk_loop(h, prologue(h + 1) if h + 1 < 8 else None)
        phA.close()
        P.barrier()
        mergedT = sb("mergedT", [128, 8, S], BF16, phM)

        phG = ExitStack()
        m1 = sb("m1", [128, 1024], F32, phG)
        m2 = sb("m2", [128, 1024], F32, phG)
        CVT = ["cvT0", "cvT1", "cvT2", "cvT3"]
        wgm = [sb(f"wgm{i}", [128, 8, 128], BF16, phG) for i in range(4)]

        def m_loads(j):
            res = []
            for q_, (W_, c_) in enumerate(((w_a, j * 128), (w_b, j * 128), (w_in, (48 + j) * 128), (w_in, (56 + j) * 128))):
                buf, tok = (wg[q_], f"wg{q_}") if j % 2 == 0 else (wgm[q_], f"wgm{q_}")
                Wv_ = W_.rearrange("(kc p) n -> p kc n", p=128)
                P.dma('sp', lambda e, Wv_=Wv_, c_=c_: e.dma_start(out=wgf[0][:], in_=Wv_[:, :, c_:c_ + 128]), w=["wgf0"])
                P.op('act', lambda e, buf=buf: e.copy(buf[:], wgf[0][:]), r=["wgf0"], w=[tok])
                res.append((buf, tok))
            return res

        mnext = m_loads(0)
        for j in range(8):
            (wa_, ta_), (wb_, tb_), (wga_, tga_), (wgb_, tgb_) = mnext
            if j + 1 < 8:
                mnext = m_loads(j + 1)
            for half in range(2):
                b = zmm(wga_, tga_, half)
                P.op('act', lambda e, b=b: e.activation(m1[:], zp[b][:], AF.Sigmoid), r=ZT[b], w=["m1"])
                b = zmm(wgb_, tgb_, half)
                P.op('act', lambda e, b=b: e.activation(m2[:], zp[b][:], AF.Sigmoid), r=ZT[b], w=["m2"])
                for tg in range(2):
                    t0 = half * 1024 + tg * 512
                    for kc in range(8):
                        P.op('pe', lambda e, tg=tg, kc=kc, t0=t0: e.matmul(gp[tg][:], wa_[:, kc, :], oaT[:, kc, t0:t0 + 512],
                                                                        start=(kc == 0), stop=(kc == 7)), r=[ta_, "oaT"], w=[GT[tg]])
                    for kc in range(8):
                        P.op('pe', lambda e, tg=tg, kc=kc, t0=t0: e.matmul(gp[2 + tg][:], wb_[:, kc, :], cvT[:, kc, t0:t0 + 512],
                                                                        start=(kc == 0), stop=(kc == 7)), r=[tb_] + CVT, w=[GT[2 + tg]])
                    P.op('dve', lambda e, tg=tg: e.tensor_tensor(m1[:, tg * 512:(tg + 1) * 512], gp[tg][:], m1[:, tg * 512:(tg + 1) * 512], ALU.mult),
                         r=[GT[tg], "m1"], w=["m1"])
                    P.op('dve', lambda e, tg=tg: e.tensor_tensor(m2[:, tg * 512:(tg + 1) * 512], gp[2 + tg][:], m2[:, tg * 512:(tg + 1) * 512], ALU.mult),
                         r=[GT[2 + tg], "m2"], w=["m2"])
                P.op('dve', lambda e, j=j, half=half: e.tensor_tensor(mergedT[:, j, half * 1024:(half + 1) * 1024], m1[:], m2[:], ALU.add),
                     r=["m1", "m2"], w=["mergedT"])
        phG.close()
        P.barrier()

        def rowform(dst_ap, vv, stack):
            dgl = [sb(f"dg{vv}_{i}", [128, 128], F32, stack) for i in range(2)]
            for j in range(8):
                b_ = j % 2
                P.op('dve', lambda e, b_=b_, j=j: e.tensor_scalar(dgl[b_][:], ident[:], cvec[:, vv, j:j + 1], None, ALU.mult),
                     r=["ident", "cvec"], w=[f"dgl{vv}_{b_}"])
                pb = 2 + b_
                P.op('pe', lambda e, b_=b_, pb=pb: e.matmul(gp[pb][:, 0:128], ones_f[:], dgl[b_][:], start=True, stop=True),
                     r=["ones_f", f"dgl{vv}_{b_}"], w=[GT[pb]])
                P.op('act', lambda e, pb=pb, j=j: e.copy(dst_ap[:, j * 128:(j + 1) * 128], gp[pb][:, 0:128]), r=[GT[pb]], w=["rowv"])

        phY = ExitStack()
        rg1 = sb("rg1", [128, 1024], F32, phY)
        rowform(rg1, V_G1, phY)
        wout = sb("wout", [128, 8, 1024], BF16, phY)
        wov = w_out.rearrange("(kc p) n -> p kc n", p=128)
        for kc in range(8):
            P.dma('pool', lambda e, kc=kc: e.dma_start(out=wout[:, kc, :], in_=wov[:, kc, :]), w=["wout"])
        xta = [sb(f"xta{i}", [128, 1024], F32, phY) for i in range(2)]
        h1a = [sb(f"h1a{i}", [128, 1024], F32, phY) for i in range(2)]
        for i in range(NT):
            b = i % 2
            P.dma('sp', lambda e, i=i, b=b: e.dma_start(out=xta[b][:], in_=x[i * 128:(i + 1) * 128, :]), w=[f"xta{b}"])
            for ng in range(2):
                for kc in range(8):
                    P.op('pe', lambda e, i=i, b=b, ng=ng, kc=kc: e.matmul(zp[b][:, ng * 512:(ng + 1) * 512], mergedT[:, kc, i * 128:(i + 1) * 128],
                                                                        wout[:, kc, ng * 512:(ng + 1) * 512], start=(kc == 0), stop=(kc == 7)),
                         r=["mergedT", "wout"], w=[ZT[b][ng]])
            P.op('dve', lambda e, b=b: e.tensor_tensor(h1a[b][:], zp[b][:], rg1[:], ALU.mult), r=ZT[b] + ["rowv"], w=[f"h1a{b}"])
            P.op('dve', lambda e, b=b: e.tensor_tensor(h1a[b][:], h1a[b][:], xta[b][:], ALU.add), r=[f"h1a{b}", f"xta{b}"], w=[f"h1a{b}"])
            P.dma('sp', lambda e, i=i, b=b: e.dma_start(out=h1s[i * 128:(i + 1) * 128, :], in_=h1a[b][:]), r=[f"h1a{b}"], w=["h1s"])
        phY.close()
        phM.close()
        P.barrier()

        rowv = sb("rowv", [128, 2, 1024], F32)
        rowform(rowv[:, 0, :], V_G2, ES)
        rowform(rowv[:, 1, :], V_FG, ES)
        RG2, RFG = rowv[:, 0, :], rowv[:, 1, :]
        wqb = sb("wqb", [128, 8, 2048], BF16)
        keysT = sb("keysT", [128, 16, 128], BF16)
        s_ = sb("s_", [128, 16, 128], F32)
        wst = s_[:].rearrange("p a b -> p (a b)")
        wqv = peer_wq.rearrange("(kc p) n -> p kc n", p=128)
        for kc in range(8):
            P.dma('pool', lambda e, kc=kc: e.dma_start(out=wqb[:, kc, :], in_=wqv[:, kc, :]), w=["wqb"])
        kv_ = peer_keys.rearrange("g n d -> n g d")
        P.dma('sp', lambda e: e.dma_start(out=s_[:], in_=kv_), w=["s_"])
        for g in range(16):
            pb = g % 4
            P.op('pe', lambda e, g=g, pb=pb: e.transpose(gp[pb][:, 0:128], wst[:, g * 128:(g + 1) * 128], ident[:]),
                 r=["s_", "ident"], w=[GT[pb]])
            P.op('act', lambda e, g=g, pb=pb: e.copy(keysT[:, g, :], gp[pb][:, 0:128]), r=[GT[pb]], w=["keysT"])

        h1 = [sb(f"h1_{i}", [128, 1024], F32) for i in range(2)]
        u2 = [sb(f"u2_{i}", [128, 1024], BF16) for i in range(2)]
        prodb = [sb(f"prodb{i}", [128, 1024], BF16) for i in range(2)]
        xn2 = sb("xn2", [128, 1024], F32)
        jk = sb("jk", [128, 1024], BF16)
        fin = sb("fin", [128, 1024], F32)
        u2T = sb("u2T", [128, 8, 128], BF16)
        qT = sb("qT", [128, 16, 128], BF16)
        s2_ = sb("s2_", [128, 16, 128], F32)
        sc_ = sb("sc_", [128, 16, 16], F32)
        idx_ = sb("idx_", [128, 16, 16], U32)
        idxf = sb("idxf", [128, 16, 16], F32)
        cand = s_[:].rearrange("p a b -> p (a b)").rearrange("p (h c) -> p h c", c=256)
        cand2 = s2_[:].rearrange("p a b -> p (a b)").rearrange("p (h c) -> p h c", c=256)
        oh = s2_[:].rearrange("p a b -> p (a b)").rearrange("p (h k a) -> p h k a", k=16, a=16)
        top_ = sb("top_", [128, 8, 16], F32)
        pos_ = sb("pos_", [128, 8, 16], U32)
        pa_ = sb("pa_", [128, 8, 16], U32)
        paf = sb("paf", [128, 8, 16], F32)
        pbf = sb("pbf", [128, 8, 16], F32)
        isel = sb("isel", [128, 128], F32)
        jsel = sb("jsel", [128, 128], F32)
        eidx = [sb(f"eidx{i}", [128, 128], U32) for i in range(2)]
        gate = [sb(f"gate{i}", [128, 128], F32) for i in range(2)]
        gsum = sb("gsum", [128, 8], F32)
        act_ = sb("act_", [128, 128], F32)
        gl_ = sb("gl_", [128, 128], F32)
        st2 = sb("st2", [128, 8], F32)
        st3 = sb("st3", [128, 8], F32)
        io16 = sb("io16", [128, 16], F32)
        NGB = 22
        GB = [sb(f"GB{i}", [128, 2048], BF16) for i in range(NGB)]
        dgs = [sb(f"dgs{i}", [128, 128], BF16) for i in range(4)]
        P.op('dve', lambda e: e.tensor_copy(io16[:], iot_i[:, 0:16]), r=["iot_i"], w=["io16"])
        NEG = -1.0e30

        def front(i):
            b = i % 2
            H1, U2, EIDX, GATE = h1[b], u2[b], eidx[b], gate[b]
            g3 = GATE[:].rearrange("p (h k) -> p h k", k=16)
            P.dma('sp', lambda e: e.dma_start(out=H1[:], in_=h1s[i * 128:(i + 1) * 128, :]), r=["h1s"], w=[f"h1_{b}"])
            yield
            P.op('dve', lambda e: e.scalar_tensor_tensor(xn2[:], H1[:], 1.0, H1[:], ALU.mult, ALU.mult, accum_out=st2[:, 0:1]),
                 r=[f"h1_{b}"], w=["xn2", "st2"])
            yield
            P.op('act', lambda e: e.activation(st2[:, 1:2], st2[:, 0:1], AF.Sqrt, scale=1.0 / D, bias=EPS), r=["st2"], w=["st2"])
            P.op('dve', lambda e: e.reciprocal(st2[:, 2:3], st2[:, 1:2]), r=["st2"], w=["st2"])
            yield
            P.op('dve', lambda e: e.tensor_scalar(xn2[:], H1[:], st2[:, 2:3], None, ALU.mult), r=[f"h1_{b}", "st2"], w=["xn2"])
            yield
            yield
            for j in range(8):
                pb = j // 4
                P.op('pe', lambda e, j=j, pb=pb: e.transpose(gp[pb][:, (j % 4) * 128:(j % 4 + 1) * 128], xn2[:, j * 128:(j + 1) * 128], ident[:]),
                     r=["xn2", "ident"], w=[GT[pb]])
            for j in range(8):
                pb = j // 4
                P.op('act', lambda e, j=j, pb=pb: e.activation(u2T[:, j, :], gp[pb][:, (j % 4) * 128:(j % 4 + 1) * 128], AF.Identity,
                                                             scale=cvec[:, V_W2, j:j + 1], bias=cvec[:, V_SH2, j:j + 1]),
                     r=[GT[pb], "cvec"], w=["u2T"])
            gpb2 = gp[2][:].bitcast(BF16)
            for j in range(8):
                P.op('pe', lambda e, j=j: e.transpose(gpb2[:, j * 128:(j + 1) * 128], u2T[:, j, :], identb[:]), r=["u2T", "identb"], w=[GT[2]])
            P.op('act', lambda e: e.copy(U2[:], gpb2), r=[GT[2]], w=[f"u2_{b}"])
            yield
            for g in range(16):
                pb = 2 + (g // 4) % 2
                for kc in range(8):
                    P.op('pe', lambda e, g=g, kc=kc, pb=pb: e.matmul(gp[pb][:, (g % 4) * 128:(g % 4 + 1) * 128], wqb[:, kc, g * 128:(g + 1) * 128],
                                                                   u2T[:, kc, :], start=(kc == 0), stop=(kc == 7)), r=["wqb", "u2T"], w=[GT[pb]])
                if g % 4 == 3:
                    P.op('act', lambda e, g=g, pb=pb: e.copy(qT[:, g - 3:g + 1, :].rearrange("p a b -> p (a b)"), gp[pb][:]), r=[GT[pb]], w=["qT"])
                    yield
            for g in range(16):
                pb = (g // 4) % 2
                P.op('pe', lambda e, g=g, pb=pb: e.matmul(gp[pb][:, (g % 4) * 128:(g % 4 + 1) * 128], qT[:, g, :], keysT[:, g, :],
                                                        start=True, stop=True), r=["qT", "keysT"], w=[GT[pb]])
                if g % 4 == 3:
                    P.op('act', lambda e, g=g, pb=pb: e.copy(s_[:, g - 3:g + 1, :].rearrange("p a b -> p (a b)"), gp[pb][:]), r=[GT[pb]], w=["s_"])
            yield
            for g in range(16):
                P.op('dve', lambda e, g=g: e.max(sc_[:, g, 0:8], s_[:, g, :]), r=["s_"], w=["sc_"])
                yield
                P.op('dve', lambda e, g=g: e.match_replace(s2_[:, g, :], sc_[:, g, 0:8], s_[:, g, :], NEG), r=["s_", "sc_"], w=["s2_"])
                yield
                P.op('dve', lambda e, g=g: e.max(sc_[:, g, 8:16], s2_[:, g, :]), r=["s2_"], w=["sc_"])
                yield
                P.op('dve', lambda e, g=g: e.max_index(idx_[:, g, 0:8], sc_[:, g, 0:8], s_[:, g, :]), r=["s_", "sc_"], w=["idx_"])
                yield
                P.op('dve', lambda e, g=g: e.max_index(idx_[:, g, 8:16], sc_[:, g, 8:16], s_[:, g, :]), r=["s_", "sc_"], w=["idx_"])
                yield
                if g % 2 == 1:
                    yield
            P.op('dve', lambda e: e.tensor_copy(idxf[:], idx_[:]), r=["idx_"], w=["idxf"])
            yield
            sc4 = sc_[:].rearrange("p (h two) k -> p h two k", two=2)
            ix4 = idxf[:].rearrange("p (h two) k -> p h two k", two=2)
            P.op('dve', lambda e: e.tensor_tensor(cand.rearrange("p h (a b) -> p h a b", b=16),
                                                  sc4[:, :, 0, :].unsqueeze(3).to_broadcast([128, 8, 16, 16]),
                                                  sc4[:, :, 1, :].unsqueeze(2).to_broadcast([128, 8, 16, 16]), ALU.add),
                 r=["sc_"], w=["s_"])
            yield
            yield
            for hh in range(8):
                P.op('dve', lambda e, hh=hh: e.max(top_[:, hh, 0:8], cand[:, hh, :]), r=["s_"], w=["top_"])
                yield
                P.op('dve', lambda e, hh=hh: e.match_replace(cand2[:, hh, :], top_[:, hh, 0:8], cand[:, hh, :], NEG), r=["s_", "top_"], w=["s2_"])
                yield
                P.op('dve', lambda e, hh=hh: e.max(top_[:, hh, 8:16], cand2[:, hh, :]), r=["s2_"], w=["top_"])
                yield
                P.op('dve', lambda e, hh=hh: e.max_index(pos_[:, hh, 0:8], top_[:, hh, 0:8], cand[:, hh, :]), r=["s_", "top_"], w=["pos_"])
                yield
                P.op('dve', lambda e, hh=hh: e.max_index(pos_[:, hh, 8:16], top_[:, hh, 8:16], cand[:, hh, :]), r=["s_", "top_"], w=["pos_"])
                yield
                yield
            P.op('dve', lambda e: e.tensor_scalar(pa_[:], pos_[:], 4, None, ALU.logical_shift_right), r=["pos_"], w=["pa_"])
            yield
            P.op('dve', lambda e: e.tensor_copy(paf[:], pa_[:]), r=["pa_"], w=["paf"])
            yield
            P.op('dve', lambda e: e.tensor_scalar(pa_[:], pos_[:], 15, None, ALU.bitwise_and), r=["pos_"], w=["pa_"])
            yield
            P.op('dve', lambda e: e.tensor_copy(pbf[:], pa_[:]), r=["pa_"], w=["pbf"])
            yield
            yield
            for which, (pf, dst) in enumerate(((paf, isel), (pbf, jsel))):
                P.op('dve', lambda e, pf=pf: e.tensor_tensor(oh, pf[:].unsqueeze(3).to_broadcast([128, 8, 16, 16]),
                                                             io16[:].unsqueeze(1).unsqueeze(1).to_broadcast([128, 8, 16, 16]), ALU.is_equal),
                     r=["paf", "pbf", "io16"], w=["s2_"])
                yield
                P.op('dve', lambda e, which=which: e.tensor_tensor(oh, oh, ix4[:, :, which, :].unsqueeze(2).to_broadcast([128, 8, 16, 16]), ALU.mult),
                     r=["s2_", "idxf"], w=["s2_"])
                yield
                P.op('dve', lambda e, dst=dst: e.tensor_reduce(dst[:], oh.rearrange("p h k a -> p (h k) a"), AX.X, ALU.add),
                     r=["s2_"], w=["isel", "jsel"])
                yield
                yield
            P.op('dve', lambda e: e.scalar_tensor_tensor(isel[:], isel[:], 128.0, jsel[:], ALU.mult, ALU.add), r=["isel", "jsel"], w=["isel"])
            yield
            P.op('dve', lambda e: e.tensor_copy(EIDX[:], isel[:]), r=["isel"], w=[f"eidx{b}"])
            yield
            P.op('dve', lambda e: e.tensor_tensor(g3, top_[:], top_[:, :, 0:1].to_broadcast([128, 8, 16]), ALU.subtract), r=["top_"], w=[f"gate{b}"])
            yield
            P.op('act', lambda e: e.activation(GATE[:], GATE[:], AF.Exp), r=[f"gate{b}"], w=[f"gate{b}"])
            P.op('dve', lambda e: e.tensor_reduce(gsum[:], g3, AX.X, ALU.add), r=[f"gate{b}"], w=["gsum"])
            yield
            P.op('dve', lambda e: e.reciprocal(gsum[:], gsum[:]), r=["gsum"], w=["gsum"])
            yield
            P.op('dve', lambda e: e.tensor_tensor(g3, g3, gsum[:].unsqueeze(2).to_broadcast([128, 8, 16]), ALU.mult), r=[f"gate{b}", "gsum"], w=[f"gate{b}"])
            yield
            yield

        def back(i, nxt):
            b = i % 2
            H1, U2, EIDX, GATE = h1[b], u2[b], eidx[b], gate[b]
            G4 = 4

            def slot_front(sl):
                k = sl % NGB
                P.dma('pool', lambda e: e.indirect_dma_start(out=GB[k][:], out_offset=None, in_=tabUV,
                                                             in_offset=bass.IndirectOffsetOnAxis(ap=EIDX[:, sl:sl + 1], axis=0)),
                      r=[f"eidx{b}"], w=[f"GB{k}"])
                if sl % 2 == 0:
                    P.op('dve', lambda e: e.scalar_tensor_tensor(jk[:], GB[k][:, 0:1024], 1.0, U2[:], ALU.mult, ALU.mult,
                                                                 accum_out=act_[:, sl:sl + 1]),
                         r=[f"GB{k}", f"u2_{b}"], w=["jk", f"act{sl}"])
                else:
                    pq = (sl // 2) % 2
                    P.op('dve', lambda e: e.tensor_tensor(prodb[pq][:], GB[k][:, 0:1024], U2[:], ALU.mult),
                         r=[f"GB{k}", f"u2_{b}"], w=[f"prodb{pq}"])
                    P.op('act', lambda e: e.activation(prodb[pq][:], prodb[pq][:], AF.Copy, accum_out=act_[:, sl:sl + 1]),
                         r=[f"prodb{pq}"], w=[f"prodb{pq}", f"act{sl}"])

            NDG = 4

            def st_gelu(sl):
                P.op('act', lambda e: e.activation(gl_[:, sl:sl + 1], act_[:, sl:sl + 1], AF.Gelu), r=[f"act{sl}"], w=[f"gl{sl}"])

            def st_gate(sl):
                P.op('dve', lambda e: e.tensor_tensor(gl_[:, sl:sl + 1], gl_[:, sl:sl + 1], GATE[:, sl:sl + 1], ALU.mult),
                     r=[f"gl{sl}", f"gate{b}"], w=[f"gl{sl}"])

            def st_diag(sl):
                k2 = sl % NDG
                P.op('act', lambda e: e.activation(dgs[k2][:], ident[:], AF.Copy, scale=gl_[:, sl:sl + 1]),
                     r=["ident", f"gl{sl}"], w=[f"dgs{k2}"])

            def st_mm(sl):
                k = sl % NGB
                k2 = sl % NDG
                for ng in range(2):
                    P.op('pe', lambda e, ng=ng: e.matmul(zp[1][:, ng * 512:(ng + 1) * 512], dgs[k2][:],
                                                       GB[k][:, 1024 + ng * 512:1024 + (ng + 1) * 512],
                                                       start=(sl == 0), stop=(sl == 127)),
                         r=[f"dgs{k2}", f"GB{k}"], w=[ZT[1][ng]])

            L1, L2, L3, L4 = 2, 4, 6, 8
            for p_ in range(128 + L4):
                if p_ < 128:
                    slot_front(p_)
                    if nxt is not None:
                        next(nxt, None)
                        if p_ % 2 == 1:
                            next(nxt, None)
                for L, fn in ((L1, st_gelu), (L2, st_gate), (L3, st_diag), (L4, st_mm)):
                    if 0 <= p_ - L < 128:
                        fn(p_ - L)
            if nxt is not None:
                for _ in nxt:
                    pass
            P.op('dve', lambda e: e.tensor_tensor(fin[:], zp[1][:], RG2, ALU.mult), r=ZT[1] + ["rowv"], w=["fin"])
            P.op('dve', lambda e: e.tensor_tensor(H1[:], H1[:], fin[:], ALU.add), r=[f"h1_{b}", "fin"], w=[f"h1_{b}"])
            P.op('dve', lambda e: e.scalar_tensor_tensor(fin[:], H1[:], 1.0, H1[:], ALU.mult, ALU.mult, accum_out=st3[:, 0:1]),
                 r=[f"h1_{b}"], w=["fin", "st3"])
            P.op('act', lambda e: e.activation(st3[:, 1:2], st3[:, 0:1], AF.Sqrt, scale=1.0 / D, bias=EPS), r=["st3"], w=["st3"])
            P.op('dve', lambda e: e.reciprocal(st3[:, 2:3], st3[:, 1:2]), r=["st3"], w=["st3"])
            P.op('dve', lambda e: e.scalar_tensor_tensor(fin[:], H1[:], st3[:, 2:3], RFG, ALU.mult, ALU.mult), r=[f"h1_{b}", "st3", "rowv"], w=["fin"])
            P.dma('sp', lambda e: e.dma_start(out=out[i * 128:(i + 1) * 128, :], in_=fin[:]), r=["fin"], w=["out"])

        for _ in front(0):
            pass
        for i in range(NT):
            back(i, front(i + 1) if i + 1 < NT else None)

        P.finish(["out"])
    return nc


_CACHE = {}


def kernel(**inputs):
    inp = {k: np.ascontiguousarray(np.asarray(v, dtype=np.float32)) for k, v in inputs.items()}
    if "nc" not in _CACHE:
        _CACHE["nc"] = build_nc()
    nc = _CACHE["nc"]
    shared = {
        "ada_w": inp["ada_w"][0], "ada_b": inp["ada_b"][0], "norm1_g": inp["norm1_g"][0], "w_in": inp["w_in"][0],
        "lb_logits": inp["lb_logits"], "hg_norm_g": inp["hg_norm_g"][0], "w_a": inp["w_a"][0],
        "conv_w": inp["conv_w"][0], "conv_b": inp["conv_b"][0], "conv_ln_g": inp["conv_ln_g"][0],
        "conv_ln_b": inp["conv_ln_b"][0], "w_b": inp["w_b"][0], "w_out": inp["w_out"][0],
        "norm2_g": inp["norm2_g"][0], "peer_wq": inp["peer_wq"][0],
        "peer_keys": inp["peer_keys"][0].reshape(16, 128, 128), "peer_u": inp["peer_u"][0],
        "peer_v": inp["peer_v"][0], "final_g": inp["final_g"],
    }
    in_maps = []
    for b in range(NCORES):
        m = dict(shared)
        m["x"] = inp["x"][b]
        m["c"] = inp["c"][b]
        in_maps.append(m)
    res = run_bass_kernel_spmd(nc, in_maps, core_ids=list(range(NCORES)))
    return np.stack([np.asarray(r["out"], dtype=np.float32) for r in res.results], axis=0)
```

```python
import numpy as np
from contextlib import ExitStack
import concourse.bass as bass
import concourse.mybir as mybir
from concourse.bass_utils import run_bass_kernel_spmd

F32 = mybir.dt.float32
BF16 = mybir.dt.bfloat16
U32 = mybir.dt.uint32
I32 = mybir.dt.int32
AF = mybir.ActivationFunctionType
ALU = mybir.AluOpType
AX = mybir.AxisListType

S = 2048
D = 1024
NT = 16
EPS = 1e-6
NCORES = 8


class Prog:
    def __init__(self, nc):
        self.nc = nc
        self.stack = ExitStack()
        self.E = dict(pe=nc.tensor, act=nc.scalar, dve=nc.vector, pool=nc.gpsimd, sp=nc.sync)
        self.sem = {k: self.stack.enter_context(nc.semaphore("sem_" + k)) for k in self.E}
        self.cnt = {k: 0 for k in self.E}
        self.known = {k: {} for k in self.E}
        self.NDS = 64
        self.dsem = [self.stack.enter_context(nc.semaphore(f"dsem{i}")) for i in range(self.NDS)]
        self.dcnt = [0] * self.NDS
        self.dnext = 0
        self.dnext_q = {}
        self.res = {}
        self.nins = 0

    def _wait(self, e, key, val):
        if val <= 0:
            return
        if e == 'pe' and key == 'pe':
            return
        if self.known[e].get(key, 0) >= val:
            return
        sem = self.sem[key] if isinstance(key, str) else self.dsem[key[1]]
        self.E[e].wait_ge(sem, val)
        self.known[e][key] = val

    def _deps(self, r, w):
        deps = {}

        def add(k, v):
            if deps.get(k, 0) < v:
                deps[k] = v
        for t in r:
            st = self.res.get(t)
            if st and st[0]:
                add(*st[0])
        for t in w:
            st = self.res.get(t)
            if st:
                if st[0]:
                    add(*st[0])
                for k, v in st[1].items():
                    add(k, v)
        return deps

    def _record(self, me, r, w):
        for t in r:
            st = self.res.setdefault(t, [None, {}])
            if st[1].get(me[0], 0) < me[1]:
                st[1][me[0]] = me[1]
        for t in w:
            self.res[t] = [me, {}]

    def op(self, e, fn, r=(), w=()):
        for k, v in self._deps(r, w).items():
            self._wait(e, k, v)
        ins = fn(self.E[e])
        self.cnt[e] += 1
        ins.then_inc(self.sem[e], 1)
        self._record((e, self.cnt[e]), r, w)
        self.nins += 1

    def dma(self, q, fn, r=(), w=()):
        for k, v in self._deps(r, w).items():
            self._wait(q, k, v)
        half = self.NDS // 2
        base = 0 if q == 'sp' else half
        i = base + self.dnext_q.get(q, 0)
        self.dnext_q[q] = (self.dnext_q.get(q, 0) + 1) % half
        self._wait(q, ('d', i), self.dcnt[i])
        ins = fn(self.E[q])
        self.dcnt[i] += 16
        ins.then_inc(self.dsem[i], 16)
        self._record((('d', i), self.dcnt[i]), r, w)
        self.nins += 1

    def barrier(self, skip_pool_dma=False, compute_only=False):
        nd = 0 if compute_only else (self.NDS // 2 if skip_pool_dma else self.NDS)
        for e in self.E:
            if skip_pool_dma and e == 'pool':
                continue
            for k in self.E:
                if k != e:
                    self._wait(e, k, self.cnt[k])
            for i in range(nd):
                self._wait(e, ('d', i), self.dcnt[i])

    def finish(self, tokens):
        for t in tokens:
            st = self.res.get(t)
            if st and st[0]:
                self._wait('sp', st[0][0], st[0][1])


def build_nc(stage=99, dbg_shape=None):
    nc = bass.Bass("TRN2", target_bir_lowering=False)
    dram = {}

    def din(name, shape):
        dram[name] = nc.dram_tensor(name, list(shape), F32, kind="ExternalInput").ap()
        return dram[name]
    x = din("x", [S, D])
    c = din("c", [D])
    ada_w = din("ada_w", [D, 6 * D])
    ada_b = din("ada_b", [6 * D])
    norm1_g = din("norm1_g", [D])
    w_in = din("w_in", [D, 8 * D])
    lb_logits = din("lb_logits", [2, D])
    hg_norm_g = din("hg_norm_g", [D])
    w_a = din("w_a", [D, D])
    conv_w = din("conv_w", [31, D])
    conv_b = din("conv_b", [D])
    conv_ln_g = din("conv_ln_g", [D])
    conv_ln_b = din("conv_ln_b", [D])
    w_b = din("w_b", [D, D])
    w_out = din("w_out", [D, D])
    norm2_g = din("norm2_g", [D])
    peer_wq = din("peer_wq", [D, 2 * D])
    peer_keys = din("peer_keys", [16, 128, 128])
    peer_u = din("peer_u", [16384, D])
    peer_v = din("peer_v", [16384, D])
    final_g = din("final_g", [D])
    out = nc.dram_tensor("out", [S, D], F32, kind="ExternalOutput").ap()
    tabUV = nc.dram_tensor("tabUV", [16384, 2048], BF16, kind="Internal").ap()
    h1s = nc.dram_tensor("h1s", [S, D], F32, kind="Internal").ap()
    dbg = None
    if dbg_shape is not None:
        dbg = nc.dram_tensor("dbg", list(dbg_shape), F32, kind="ExternalOutput").ap()

    P = Prog(nc)
    ES = ExitStack()

    def sb(name, shape, dt=F32, stack=None):
        return (stack or ES).enter_context(nc.sbuf_tensor(name, list(shape), dt))

    def ps(name, shape, dt=F32):
        return ES.enter_context(nc.psum_tensor(name, list(shape), dt))

    with P.stack, ES:
        zp = [ps("zp0", [128, 1024]), ps("zp1", [128, 1024])]
        gp = [ps(f"gp{i}", [128, 512]) for i in range(4)]
        ZT = [["zp0a", "zp0b"], ["zp1a", "zp1b"]]
        GT = ["gp0", "gp1", "gp2", "gp3"]

        ident = sb("ident", [128, 128], F32)
        identb = sb("identb", [128, 128], BF16)
        ones_b = sb("ones_b", [128, 128], BF16)
        onesD_b = sb("onesD_b", [128, 128], BF16)
        ones_f = sb("ones_f", [128, 128], F32)
        iot_i = sb("iot_i", [128, 128], I32)
        iop_i = sb("iop_i", [128, 128], I32)
        P.op('pool', lambda e: e.iota(iot_i[:], [[1, 128]], base=0, channel_multiplier=0), w=["iot_i"])
        P.op('pool', lambda e: e.iota(iop_i[:], [[0, 128]], base=0, channel_multiplier=1), w=["iop_i"])
        P.op('dve', lambda e: e.tensor_tensor(ident[:], iot_i[:], iop_i[:], ALU.is_equal), r=["iot_i", "iop_i"], w=["ident"])
        P.op('dve', lambda e: e.tensor_copy(identb[:], ident[:]), r=["ident"], w=["identb"])
        P.op('dve', lambda e: e.memset(ones_b[:], 1.0 / 128), w=["ones_b"])
        P.op('dve', lambda e: e.memset(onesD_b[:], 1.0 / 1024), w=["onesD_b"])
        P.op('dve', lambda e: e.memset(ones_f[:], 1.0), w=["ones_f"])

        stg = sb("stg", [128, 128], F32)
        rows = [(c, 8), (ada_b, 48), (norm1_g, 8), (lb_logits[0, :], 8), (lb_logits[1, :], 8), (hg_norm_g, 8),
                (conv_b, 8), (conv_ln_g, 8), (conv_ln_b, 8), (norm2_g, 8), (final_g, 8)]
        r0 = 0
        offs = []
        for apx, n in rows:
            offs.append(r0)
            P.dma('sp', lambda e, apx=apx, r0=r0, n=n: e.dma_start(out=stg[r0:r0 + n, :], in_=apx.rearrange("(j p) -> j p", p=128)),
                  w=["stg"])
            r0 += n
        assert r0 == 128
        (O_C, O_ADAB, O_N1G, O_LB0, O_LB1, O_HGG, O_CB, O_LNG, O_LNB, O_N2G, O_FG) = offs
        colp = sb("colp", [128, 128], F32)
        P.op('pe', lambda e: e.transpose(gp[0][:, 0:128], stg[:], ident[:]), r=["stg", "ident"], w=[GT[0]])
        P.op('dve', lambda e: e.tensor_copy(colp[:], gp[0][:, 0:128]), r=[GT[0]], w=["colp"])
        cw = sb("cw", [128, 256], F32)
        stg2 = sb("stg2", [128, 2, 128], F32)
        P.op('dve', lambda e: e.memset(stg2[:], 0.0), w=["stg2"])
        cwv = conv_w.rearrange("k (j p) -> (k j) p", p=128)
        P.dma('sp', lambda e: e.dma_start(out=stg2[:, 0, :], in_=cwv[0:128, :]), w=["stg2"])
        P.dma('sp', lambda e: e.dma_start(out=stg2[0:120, 1, :], in_=cwv[128:248, :]), w=["stg2"])
        for hh in range(2):
            P.op('pe', lambda e, hh=hh: e.transpose(gp[1][:, hh * 128:(hh + 1) * 128], stg2[:, hh, :], ident[:]),
                 r=["stg2", "ident"], w=[GT[1]])
        P.op('dve', lambda e: e.tensor_copy(cw[:], gp[1][:, 0:256]), r=[GT[1]], w=["cw"])

        sc_col = sb("sc_col", [128, 8], F32)
        ada_col = sb("ada_col", [128, 48], F32)
        NV = 12
        cvec = sb("cvec", [128, NV, 8], F32)
        phM = ExitStack()
        uT = sb("uT", [128, 8, S], BF16, phM)
        wg = [sb(f"wg{i}", [128, 8, 128], BF16, phM) for i in range(4)]
        wgf = [sb(f"wgf{i}", [128, 8, 128], F32, phM) for i in range(1)]
        oaT = sb("oaT", [128, 8, S], BF16, phM)
        NCB = 8
        cbuf = [oaT[:, i, :].rearrange("p (r d) -> p r d", r=2) for i in range(NCB)]
        tU = peer_u.rearrange("(c p r) d -> c p r d", p=128, r=2)
        tV = peer_v.rearrange("(c p r) d -> c p r d", p=128, r=2)
        tO = tabUV.rearrange("(c p r) d -> c p r d", p=128, r=2)
        jobs = [(tU, c, 0) for c in range(64)] + [(tV, c, 1024) for c in range(64)]

        def conv_store(n):
            tv_, c_, off_ = jobs[n]
            P.dma('pool', lambda e: e.dma_start(out=tO[c_][:, :, off_:off_ + 1024], in_=cbuf[n % NCB]), r=[f"cbuf{n % NCB}"], w=["tabUV"])
        for n, (tv_, c_, off_) in enumerate(jobs):
            P.dma('pool', lambda e, tv_=tv_, c_=c_, n=n: e.dma_start(out=cbuf[n % NCB], in_=tv_[c_]), w=[f"cbuf{n % NCB}"])
            if n >= 4:
                conv_store(n - 4)
        for n in range(len(jobs) - 4, len(jobs)):
            conv_store(n)

        P.op('act', lambda e: e.activation(sc_col[:], colp[:, O_C:O_C + 8], AF.Silu), r=["colp"], w=["sc_col"])

        with ExitStack() as st_ada:
            awt = [sb(f"awt{i}", [128, 8, 512], F32, st_ada) for i in range(2)]
            awv = ada_w.rearrange("(kc p) n -> p kc n", p=128)
            for g4 in range(12):
                bi = g4 % 2
                P.dma('sp', lambda e, g4=g4, bi=bi: e.dma_start(out=awt[bi][:], in_=awv[:, :, g4 * 512:(g4 + 1) * 512]),
                      w=[f"awt{bi}"])
                for gg in range(4):
                    col = g4 * 4 + gg
                    for kc in range(8):
                        P.op('pe', lambda e, bi=bi, gg=gg, kc=kc, col=col: e.matmul(
                            gp[2][:, col:col + 1], awt[bi][:, kc, gg * 128:(gg + 1) * 128], sc_col[:, kc:kc + 1],
                            start=(kc == 0), stop=(kc == 7)), r=[f"awt{bi}", "sc_col"], w=[GT[2]])
            P.op('dve', lambda e: e.tensor_tensor(ada_col[:], gp[2][:, 0:48], colp[:, O_ADAB:O_ADAB + 48], ALU.add),
                 r=[GT[2], "colp"], w=["ada_col"])
        P.barrier(skip_pool_dma=True)
        (V_W1, V_SH1, V_W2, V_SH2, V_G1, V_G2, V_LB, V_OML, V_NOML, V_FG, V_TMP, V_TMP2) = range(NV)
        A_SH1, A_SC1, A_G1, A_SH2, A_SC2, A_G2 = [ada_col[:, i * 8:(i + 1) * 8] for i in range(6)]
        R_ = ["ada_col", "colp", "cvec"]
        P.op('dve', lambda e: e.scalar_tensor_tensor(cvec[:, V_W1, :], A_SC1, 1.0, colp[:, O_N1G:O_N1G + 8], ALU.add, ALU.mult), r=R_, w=["cvec"])
        P.op('dve', lambda e: e.tensor_copy(cvec[:, V_SH1, :], A_SH1), r=R_, w=["cvec"])
        P.op('dve', lambda e: e.scalar_tensor_tensor(cvec[:, V_W2, :], A_SC2, 1.0, colp[:, O_N2G:O_N2G + 8], ALU.add, ALU.mult), r=R_, w=["cvec"])
        P.op('dve', lambda e: e.tensor_copy(cvec[:, V_SH2, :], A_SH2), r=R_, w=["cvec"])
        P.op('dve', lambda e: e.tensor_copy(cvec[:, V_G1, :], A_G1), r=R_, w=["cvec"])
        P.op('dve', lambda e: e.tensor_copy(cvec[:, V_G2, :], A_G2), r=R_, w=["cvec"])
        P.op('dve', lambda e: e.tensor_copy(cvec[:, V_FG, :], colp[:, O_FG:O_FG + 8]), r=R_, w=["cvec"])
        P.op('dve', lambda e: e.tensor_tensor(cvec[:, V_TMP, :], colp[:, O_LB0:O_LB0 + 8], colp[:, O_LB1:O_LB1 + 8], ALU.subtract), r=R_, w=["cvec"])
        P.op('act', lambda e: e.activation(cvec[:, V_LB, :], cvec[:, V_TMP, :], AF.Sigmoid), r=["cvec"], w=["cvec"])
        P.op('dve', lambda e: e.tensor_scalar(cvec[:, V_OML, :], cvec[:, V_LB, :], -1.0, 1.0, ALU.mult, ALU.add), r=["cvec"], w=["cvec"])
        P.op('dve', lambda e: e.tensor_scalar(cvec[:, V_NOML, :], cvec[:, V_OML, :], -1.0, None, ALU.mult), r=["cvec"], w=["cvec"])

        if stage == 0:
            P.dma('sp', lambda e: e.dma_start(out=dbg[0:128, 0:NV * 8], in_=cvec[:].rearrange("p a b -> p (a b)")), r=["cvec"], w=["dbg"])
            P.dma('sp', lambda e: e.dma_start(out=dbg[128:256, :], in_=rowv[:, 0, :]), r=["rowv"], w=["dbg"])
            P.dma('sp', lambda e: e.dma_start(out=dbg[256:384, 0:256], in_=cw[:]), r=["cw"], w=["dbg"])
            P.dma('sp', lambda e: e.dma_start(out=dbg[384:512, 0:48], in_=ada_col[:]), r=["ada_col"], w=["dbg"])
            P.finish(["dbg"])
            return nc


        ph1 = ExitStack()
        xt = [sb(f"xt{i}", [128, 1024], F32, ph1) for i in range(2)]
        xn = [sb(f"xn{i}", [128, 1024], F32, ph1) for i in range(2)]
        junk = sb("junk", [128, 1024], F32, ph1)
        st1 = sb("st1", [128, 2, 4], F32, ph1)
        for i in range(NT):
            b = i % 2
            P.dma('sp', lambda e, i=i, b=b: e.dma_start(out=xt[b][:], in_=x[i * 128:(i + 1) * 128, :]), w=[f"xt{b}"])
            P.op('dve', lambda e, b=b: e.scalar_tensor_tensor(junk[:], xt[b][:], 1.0, xt[b][:], ALU.mult, ALU.mult,
                                                              accum_out=st1[:, b, 0:1]), r=[f"xt{b}"], w=["junk", f"st1{b}"])
            P.op('act', lambda e, b=b: e.activation(st1[:, b, 1:2], st1[:, b, 0:1], AF.Sqrt, scale=1.0 / D, bias=EPS),
                 r=[f"st1{b}"], w=[f"st1{b}"])
            P.op('dve', lambda e, b=b: e.reciprocal(st1[:, b, 2:3], st1[:, b, 1:2]), r=[f"st1{b}"], w=[f"st1{b}"])
            P.op('dve', lambda e, b=b: e.tensor_scalar(xn[b][:], xt[b][:], st1[:, b, 2:3], None, ALU.mult),
                 r=[f"xt{b}", f"st1{b}"], w=[f"xn{b}"])
            for j in range(8):
                pb = j // 4
                P.op('pe', lambda e, b=b, j=j, pb=pb: e.transpose(gp[pb][:, (j % 4) * 128:(j % 4 + 1) * 128],
                                                               xn[b][:, j * 128:(j + 1) * 128], ident[:]),
                     r=[f"xn{b}", "ident"], w=[GT[pb]])
            for j in range(8):
                pb = j // 4
                P.op('act', lambda e, i=i, j=j, pb=pb: e.activation(
                    uT[:, j, i * 128:(i + 1) * 128], gp[pb][:, (j % 4) * 128:(j % 4 + 1) * 128], AF.Identity,
                    scale=cvec[:, V_W1, j:j + 1], bias=cvec[:, V_SH1, j:j + 1]), r=[GT[pb], "cvec"], w=["uT"])
        ph1.close()
        P.barrier(skip_pool_dma=True)

        NWG = 4
        wctr = [0]

        def load_wg(W, col0):
            b = wctr[0] % NWG
            bf = 0
            wctr[0] += 1
            Wv = W.rearrange("(kc p) n -> p kc n", p=128)
            P.dma('sp', lambda e: e.dma_start(out=wgf[bf][:], in_=Wv[:, :, col0:col0 + 128]), w=[f"wgf{bf}"])
            P.op('act', lambda e: e.copy(wg[b][:], wgf[bf][:]), r=[f"wgf{bf}"], w=[f"wg{b}"])
            return wg[b], f"wg{b}"

        zctr = [0]

        def zmm(wt, wtok, half):
            b = zctr[0] % 2
            zctr[0] += 1
            for tg in range(2):
                t0 = half * 1024 + tg * 512
                for kc in range(8):
                    P.op('pe', lambda e, tg=tg, kc=kc, t0=t0: e.matmul(zp[b][:, tg * 512:(tg + 1) * 512], wt[:, kc, :],
                                                                    uT[:, kc, t0:t0 + 512], start=(kc == 0), stop=(kc == 7)),
                         r=[wtok, "uT"], w=[ZT[b][tg]])
            return b

        if stage == 1:
            dbt = sb("dbt", [128, 1024], F32)
            for j in range(8):
                P.op('dve', lambda e, j=j: e.tensor_copy(dbt[:], uT[:, j, 0:1024]), r=["uT"], w=["dbt"])
                P.dma('sp', lambda e, j=j: e.dma_start(out=dbg[j * 128:(j + 1) * 128, :], in_=dbt[:]), r=["dbt"], w=["dbg"])
            P.finish(["dbg"])
            return nc

        cvT = sb("cvT", [128, 8, S], BF16, phM)
        phB = ExitStack()
        glu = sb("glu", [128, 8, 32 + S], BF16, phB)
        PADL = 32
        dgc = [sb(f"dgc{i}", [128, 31, 128], BF16, phB) for i in range(2)]
        sgb = [sb(f"sgb{i}", [128, 1024], F32, phB) for i in range(2)]
        sqt = [sb(f"sqt{i}", [128, 512], BF16, phB) for i in range(2)]
        lnt = [sb(f"lnt{i}", [128, 512], F32, phB) for i in range(4)]
        for j in range(8):
            P.op('dve', lambda e, j=j: e.memset(glu[:, j, 0:PADL], 0.0), w=[f"glu{j}"])
        def b_loads(j):
            return load_wg(w_in, (32 + j) * 128), load_wg(w_in, (40 + j) * 128)

        def b_diag(j):
            dj = dgc[j % 2]
            for kk in range(31):
                P.op('dve', lambda e, kk=kk, dj=dj: e.tensor_scalar(dj[:, kk, :], ident[:], cw[:, kk * 8 + j:kk * 8 + j + 1], None, ALU.mult),
                     r=["ident", "cw"], w=[f"dgc{j % 2}"])

        wnext = b_loads(0)
        b_diag(0)
        for j in range(8):
            (wa_, ta_), (wb_, tb_) = wnext
            if j + 1 < 8:
                wnext = b_loads(j + 1)
            for half in range(2):
                bb = zmm(wb_, tb_, half)
                sb_ = sgb[half]
                P.op('act', lambda e, bb=bb, sb_=sb_: e.activation(sb_[:], zp[bb][:], AF.Sigmoid), r=ZT[bb], w=[f"sgb{half}"])
                ba = zmm(wa_, ta_, half)
                P.op('dve', lambda e, ba=ba, sb_=sb_, j=j, half=half: e.tensor_tensor(
                    glu[:, j, PADL + half * 1024:PADL + (half + 1) * 1024], zp[ba][:], sb_[:], ALU.mult),
                    r=ZT[ba] + [f"sgb{half}"], w=[f"glu{j}"])
            if j + 1 < 8:
                b_diag(j + 1)
            dj = dgc[j % 2]
            for tg in range(4):
                t0 = tg * 512
                pb = tg % 2
                for kk in range(31):
                    P.op('pe', lambda e, j=j, kk=kk, pb=pb, t0=t0, dj=dj: e.matmul(
                        gp[pb][:], dj[:, kk, :], glu[:, j, PADL - 30 + t0 + kk:PADL - 30 + t0 + kk + 512],
                        start=(kk == 0), stop=(kk == 30)), r=[f"dgc{j % 2}", f"glu{j}"], w=[GT[pb]])
                P.op('act', lambda e, j=j, pb=pb, t0=t0: e.activation(cvT[:, j, t0:t0 + 512], gp[pb][:], AF.Identity,
                                                                    bias=colp[:, O_CB + j:O_CB + j + 1]), r=[GT[pb], "colp"], w=[f"cvT{tg}"])
        if stage == 21:
            dbt = sb("dbt", [128, 1024], F32, phB)
            for j in range(8):
                P.op('dve', lambda e, j=j: e.tensor_copy(dbt[:], glu[:, j, PADL:PADL + 1024]), r=[f"glu{j}"], w=["dbt"])
                P.dma('sp', lambda e, j=j: e.dma_start(out=dbg[j * 128:(j + 1) * 128, :], in_=dbt[:]), r=["dbt"], w=["dbg"])
            for j in range(8):
                P.op('dve', lambda e, j=j: e.tensor_copy(dbt[:], cvT[:, j, 0:1024]), r=["cvT0", "cvT1"], w=["dbt"])
                P.dma('sp', lambda e, j=j: e.dma_start(out=dbg[1024 + j * 128:1024 + (j + 1) * 128, :], in_=dbt[:]), r=["dbt"], w=["dbg"])
            P.finish(["dbg"])
            phB.close()
            return nc
        for tg in range(4):
            t0 = tg * 512
            for j in range(8):
                P.op('pe', lambda e, j=j, t0=t0: e.matmul(gp[2][:], onesD_b[:], cvT[:, j, t0:t0 + 512], start=(j == 0), stop=(j == 7)),
                     r=["onesD_b", f"cvT{tg}"], w=[GT[2]])
            for j in range(8):
                P.op('act', lambda e, j=j, t0=t0: e.activation(sqt[j % 2][:], cvT[:, j, t0:t0 + 512], AF.Square), r=[f"cvT{tg}"], w=[f"sqt{j % 2}"])
                P.op('pe', lambda e, j=j, t0=t0: e.matmul(gp[3][:], onesD_b[:], sqt[j % 2][:], start=(j == 0), stop=(j == 7)),
                     r=["onesD_b", f"sqt{j % 2}"], w=[GT[3]])
            mean_, var_, rstd_, tmp_ = lnt
            P.op('act', lambda e: e.copy(mean_[:], gp[2][:]), r=[GT[2]], w=["lnt0"])
            P.op('dve', lambda e: e.tensor_tensor(var_[:], mean_[:], mean_[:], ALU.mult), r=["lnt0"], w=["lnt1"])
            P.op('dve', lambda e: e.tensor_tensor(var_[:], gp[3][:], var_[:], ALU.subtract), r=[GT[3], "lnt1"], w=["lnt1"])
            P.op('dve', lambda e: e.tensor_scalar(var_[:], var_[:], 0.0, None, ALU.max), r=["lnt1"], w=["lnt1"])
            P.op('act', lambda e: e.activation(rstd_[:], var_[:], AF.Sqrt, bias=EPS), r=["lnt1"], w=["lnt2"])
            P.op('dve', lambda e: e.reciprocal(rstd_[:], rstd_[:]), r=["lnt2"], w=["lnt2"])
            for j in range(8):
                P.op('dve', lambda e, j=j, t0=t0: e.tensor_tensor(tmp_[:], cvT[:, j, t0:t0 + 512], mean_[:], ALU.subtract),
                     r=[f"cvT{tg}", "lnt0"], w=["lnt3"])
                P.op('dve', lambda e: e.tensor_tensor(tmp_[:], tmp_[:], rstd_[:], ALU.mult), r=["lnt3", "lnt2"], w=["lnt3"])
                P.op('act', lambda e, j=j, t0=t0: e.activation(cvT[:, j, t0:t0 + 512], tmp_[:], AF.Silu,
                                                             scale=colp[:, O_LNG + j:O_LNG + j + 1], bias=colp[:, O_LNB + j:O_LNB + j + 1]),
                     r=["lnt3", "colp"], w=[f"cvT{tg}"])
        phB.close()
        P.barrier()

        if stage == 2:
            dbt = sb("dbt", [128, 1024], F32)
            for j in range(8):
                P.op('dve', lambda e, j=j: e.tensor_copy(dbt[:], cvT[:, j, 0:1024]), r=["cvT0","cvT1","cvT2","cvT3"], w=["dbt"])
                P.dma('sp', lambda e, j=j: e.dma_start(out=dbg[j * 128:(j + 1) * 128, :], in_=dbt[:]), r=["dbt"], w=["dbg"])
            for j in range(8):
                P.op('dve', lambda e, j=j: e.tensor_copy(dbt[:], cvT[:, j, 1024:2048]), r=["cvT0","cvT1","cvT2","cvT3"], w=["dbt"])
                P.dma('sp', lambda e, j=j: e.dma_start(out=dbg[1024 + j * 128:1024 + (j + 1) * 128, :], in_=dbt[:]), r=["dbt"], w=["dbg"])
            P.finish(["dbg"])
            return nc


        phA = ExitStack()
        hA = sb("hA", [128, S], F32, phA)
        hB = sb("hB", [128, S], F32, phA)
        hC = [sb(f"hC{i}", [128, S], F32, phA) for i in range(2)]
        msk = sb("msk", [128, S], BF16, phA)
        qt = [sb(f"qt{i}", [128, S], BF16, phA) for i in range(2)]
        kt = [sb(f"kt{i}", [128, S], BF16, phA) for i in range(2)]
        vT = [sb(f"vT{i}", [128, S], BF16, phA) for i in range(2)]
        sg = [sb(f"sg{i}", [128, S], BF16, phA) for i in range(2)]
        vtok2 = [sb(f"vtok{i}", [32, 8, 128], BF16, phA) for i in range(2)]
        ktok = sb("ktok", [32, 8, 128], BF16, phA)
        Sall = sb("Sall", [128, 9, 128], BF16, phA)
        Rst2 = [sb(f"Rst{i}", [128, 128], F32, phA) for i in range(2)]
        nhalf = sb("nhalf", [128, 256], F32, phA)
        msb = sb("msb", [128, 256], F32, phA)
        PT2 = [sb(f"PT{i}", [32, 8, 32], BF16, phA) for i in range(2)]
        tri = sb("tri", [32, 32], F32, phA)
        osq = sb("osq", [128, 256], BF16, phA)
        lsc = sb("lsc", [128, 3, 256], F32, phA)
        rs_, tmpo, hOq = lsc[:, 0, :], lsc[:, 1, :], lsc[:, 2, :]
        hOi = hA[:].bitcast(I32)
        P.op('pool', lambda e: e.iota(hOi, [[1, S]], base=0, channel_multiplier=0), w=["hA"])
        P.op('dve', lambda e: e.tensor_scalar(hOi, hOi, 31, None, ALU.bitwise_and), r=["hA"], w=["hA"])
        P.op('dve', lambda e: e.tensor_scalar(msk[:], hOi, 0.0, None, ALU.is_gt), r=["hA"], w=["msk"])
        P.op('dve', lambda e: e.tensor_tensor(tri[:], iot_i[0:32, 0:32], iop_i[0:32, 0:32], ALU.is_ge), r=["iot_i", "iop_i"], w=["tri"])
        gpb = [g_[:].bitcast(BF16) for g_ in gp]
        P.op('dve', lambda e: e.memset(nhalf[:], -0.5), w=["nhalf"])

        def zmm0(wt, wtok, half):
            for tg in range(2):
                t0 = half * 1024 + tg * 512
                for kc in range(8):
                    P.op('pe', lambda e, tg=tg, kc=kc, t0=t0: e.matmul(zp[0][:, tg * 512:(tg + 1) * 512], wt[:, kc, :],
                                                                    uT[:, kc, t0:t0 + 512], start=(kc == 0), stop=(kc == 7)),
                         r=[wtok, "uT"], w=[ZT[0][tg]])

        def prologue(h):
            p = h % 2
            HC, QT, KT, VT, SG = hC[p], qt[p], kt[p], vT[p], sg[p]
            wf_, tf_ = load_wg(w_in, (8 + h) * 128)
            wq_, tq_ = load_wg(w_in, h * 128)
            wi_, ti_ = load_wg(w_in, (16 + h) * 128)
            wg_, tg_ = load_wg(w_in, (24 + h) * 128)
            for half in range(2):
                zmm0(wf_, tf_, half)
                P.op('act', lambda e, half=half: e.activation(hA[:, half * 1024:(half + 1) * 1024], zp[0][:], AF.Sigmoid),
                     r=ZT[0], w=["hA"])
                yield
            P.op('act', lambda e: e.activation(hB[:], hA[:], AF.Ln, scale=cvec[:, V_OML, h:h + 1], bias=cvec[:, V_LB, h:h + 1]),
                 r=["hA", "cvec"], w=["hB"])
            P.op('dve', lambda e: e.tensor_scalar(hA[:], hA[:], cvec[:, V_NOML, h:h + 1], cvec[:, V_OML, h:h + 1], ALU.mult, ALU.add),
                 r=["hA", "cvec"], w=["hA"])
            P.op('dve', lambda e: e.tensor_tensor_scan(HC[:], msk[:], hB[:], 0.0, ALU.mult, ALU.add), r=["msk", "hB"], w=[f"hC{p}"])
            P.op('act', lambda e: e.activation(hB[:], HC[:], AF.Exp, scale=-1.0), r=[f"hC{p}"], w=["hB"])
            P.op('dve', lambda e: e.tensor_tensor(KT[:], hA[:], hB[:], ALU.mult), r=["hA", "hB"], w=[f"kt{p}"])
            P.op('act', lambda e: e.activation(HC[:], HC[:], AF.Exp), r=[f"hC{p}", "hB"], w=[f"hC{p}"])
            for half in range(2):
                zmm0(wq_, tq_, half)
                P.op('dve', lambda e, half=half: e.tensor_tensor(QT[:, half * 1024:(half + 1) * 1024], zp[0][:],
                                                               HC[:, half * 1024:(half + 1) * 1024], ALU.mult),
                     r=ZT[0] + [f"hC{p}"], w=[f"qt{p}"])
                yield
            for half in range(2):
                zmm0(wi_, ti_, half)
                P.op('act', lambda e, half=half: e.copy(VT[:, half * 1024:(half + 1) * 1024], zp[0][:]), r=ZT[0], w=[f"vT{p}"])
                yield
            for half in range(2):
                zmm0(wg_, tg_, half)
                P.op('act', lambda e, half=half: e.activation(SG[:, half * 1024:(half + 1) * 1024], zp[0][:], AF.Silu),
                     r=ZT[0], w=[f"sg{p}"])
                yield

        def chunk_loop(h, nxt):
            p = h % 2
            HC, QT, KT, VT, SG = hC[p], qt[p], kt[p], vT[p], sg[p]
            tHC, tQT, tKT, tVT, tSG = f"hC{p}", f"qt{p}", f"kt{p}", f"vT{p}", f"sg{p}"
            P.op('dve', lambda e: e.memset(Sall[:, 0, :], 0.0), w=["Sall"])

            def g_T_SC(qd):
                c0 = qd * 8
                for cc in range(8):
                    t0 = (c0 + cc) * 32
                    P.op('pe', lambda e, cc=cc, t0=t0: e.transpose(gpb[0][0:32, cc * 128:(cc + 1) * 128],
                                                                 KT[:, t0:t0 + 32], identb[:]), r=[tKT, "identb"], w=[GT[0]])
                    P.op('pe', lambda e, cc=cc, t0=t0: e.transpose(gpb[2][0:32, cc * 128:(cc + 1) * 128],
                                                                 VT[:, t0:t0 + 32], identb[:]), r=[tVT, "identb"], w=[GT[2]])
                for cc in range(8):
                    t0 = (c0 + cc) * 32
                    P.op('pe', lambda e, cc=cc, t0=t0: e.matmul(zp[1][0:32, cc * 32:(cc + 1) * 32], KT[:, t0:t0 + 32], QT[:, t0:t0 + 32],
                                                              start=True, stop=True), r=[tKT, tQT], w=[ZT[1][0]])

            def g_evac_mask(qd):
                pv = qd % 2
                P.op('act', lambda e: e.copy(ktok[:].rearrange("p a b -> p (a b)"), gpb[0][0:32, :]), r=[GT[0]], w=["ktok"])
                P.op('dve', lambda e: e.tensor_copy(vtok2[pv][:].rearrange("p a b -> p (a b)"), gpb[2][0:32, :]), r=[GT[2]], w=[f"vtok{pv}"])
                P.op('dve', lambda e: e.tensor_tensor(PT2[pv][:], zp[1][0:32, 0:256].rearrange("p (c t) -> p c t", t=32),
                                                      tri[:].unsqueeze(1).to_broadcast([32, 8, 32]), ALU.mult),
                     r=[ZT[1][0], "tri"], w=[f"PT{pv}"])

            def norm_tail(qd):
                q0 = qd * 256
                P.op('pe', lambda e: e.matmul(zp[1][:, 768:1024], ones_b[:], osq[:], start=True, stop=True), r=["ones_b", "osq"], w=[ZT[1][1]])
                P.op('act', lambda e: e.activation(rs_, zp[1][:, 768:1024], AF.Sqrt, bias=EPS), r=[ZT[1][1]], w=["rs_"])
                P.op('dve', lambda e: e.reciprocal(rs_, rs_), r=["rs_"], w=["rs_"])
                P.op('dve', lambda e: e.tensor_tensor(tmpo, hOq, rs_, ALU.mult), r=["hOq", "rs_"], w=["tmpo"])
                P.op('dve', lambda e: e.scalar_tensor_tensor(oaT[:, h, q0:q0 + 256], tmpo, colp[:, O_HGG + h:O_HGG + h + 1],
                                                             SG[:, q0:q0 + 256], ALU.mult, ALU.mult),
                     r=["tmpo", "colp", tSG], w=["oaT"])

            g_T_SC(0)
            g_evac_mask(0)
            for qd in range(8):
                c0 = qd * 8
                pv = qd % 2
                VTK, PTK = vtok2[pv], PT2[pv]
                for cc in range(8):
                    P.op('pe', lambda e, cc=cc: e.matmul(gp[1 + 2 * (cc // 4)][:, (cc % 4) * 128:(cc % 4 + 1) * 128], ktok[:, cc, :], VTK[:, cc, :],
                                                       start=True, stop=True), r=["ktok", f"vtok{pv}"], w=[GT[1 + 2 * (cc // 4)]])
                if qd < 7:
                    g_T_SC(qd + 1)
                for cc in range(8):
                    c = c0 + cc
                    kvap = gp[1 + 2 * (cc // 4)][:, (cc % 4) * 128:(cc % 4 + 1) * 128]
                    Rc, Rp = Rst2[c % 2], Rst2[(c + 1) % 2]
                    if c == 0:
                        P.op('dve', lambda e, kvap=kvap, Rc=Rc: e.tensor_copy(Rc[:], kvap), r=[GT[1 + 2 * (cc // 4)]], w=[f"Rst{c % 2}"])
                    else:
                        dprev = HC[:, 32 * (c - 1) + 31:32 * (c - 1) + 32]
                        P.op('dve', lambda e, kvap=kvap, dprev=dprev, Rc=Rc, Rp=Rp: e.scalar_tensor_tensor(Rc[:], Rp[:], dprev, kvap, ALU.mult, ALU.add),
                             r=[GT[1 + 2 * (cc // 4)], f"Rst{(c + 1) % 2}", tHC], w=[f"Rst{c % 2}"])
                    dcur = HC[:, 32 * c + 31:32 * c + 32]
                    P.op('act', lambda e, cc=cc, dcur=dcur, Rc=Rc: e.activation(Sall[:, cc + 1, :], Rc[:], AF.Identity, scale=dcur),
                         r=[f"Rst{c % 2}", tHC], w=["Sall"])
                if qd < 7:
                    g_evac_mask(qd + 1)
                if qd >= 1:
                    norm_tail(qd - 1)
                if nxt is not None:
                    next(nxt, None)
                for cc in range(8):
                    t0 = (c0 + cc) * 32
                    P.op('pe', lambda e, cc=cc: e.matmul(zp[1][:, 512 + cc * 32:512 + (cc + 1) * 32], VTK[:, cc, :], PTK[:, cc, :],
                                                       start=True, stop=False), r=[f"vtok{pv}", f"PT{pv}"], w=[ZT[1][1]])
                    P.op('pe', lambda e, cc=cc, t0=t0: e.matmul(zp[1][:, 512 + cc * 32:512 + (cc + 1) * 32], Sall[:, cc, :], QT[:, t0:t0 + 32],
                                                              start=False, stop=True), r=["Sall", tQT], w=[ZT[1][1]])
                if qd < 7:
                    P.op('dve', lambda e: e.tensor_copy(Sall[:, 0, :], Sall[:, 8, :]), r=["Sall"], w=["Sall"])
                P.op('act', lambda e: e.copy(hOq, zp[1][:, 512:768]), r=[ZT[1][1]], w=["hOq"])
                P.op('act', lambda e: e.activation(osq[:], zp[1][:, 512:768], AF.Square), r=[ZT[1][1]], w=["osq"])
            norm_tail(7)
            if nxt is not None:
                for _ in nxt:
                    pass

        for _ in prologue(0):
            pass
        for h in range(8):
            chunk_loop(h, prologue(h + 1) if h + 1 < 8 else None)
        phA.close()
        P.barrier()
        mergedT = sb("mergedT", [128, 8, S], BF16, phM)

        phG = ExitStack()
        m1 = sb("m1", [128, 1024], F32, phG)
        m2 = sb("m2", [128, 1024], F32, phG)
        CVT = ["cvT0", "cvT1", "cvT2", "cvT3"]
        wgm = [sb(f"wgm{i}", [128, 8, 128], BF16, phG) for i in range(4)]

        def m_loads(j):
            res = []
            for q_, (W_, c_) in enumerate(((w_a, j * 128), (w_b, j * 128), (w_in, (48 + j) * 128), (w_in, (56 + j) * 128))):
                buf, tok = (wg[q_], f"wg{q_}") if j % 2 == 0 else (wgm[q_], f"wgm{q_}")
                Wv_ = W_.rearrange("(kc p) n -> p kc n", p=128)
                P.dma('sp', lambda e, Wv_=Wv_, c_=c_: e.dma_start(out=wgf[0][:], in_=Wv_[:, :, c_:c_ + 128]), w=["wgf0"])
                P.op('act', lambda e, buf=buf: e.copy(buf[:], wgf[0][:]), r=["wgf0"], w=[tok])
                res.append((buf, tok))
            return res

        mnext = m_loads(0)
        for j in range(8):
            (wa_, ta_), (wb_, tb_), (wga_, tga_), (wgb_, tgb_) = mnext
            if j + 1 < 8:
                mnext = m_loads(j + 1)
            for half in range(2):
                b = zmm(wga_, tga_, half)
                P.op('act', lambda e, b=b: e.activation(m1[:], zp[b][:], AF.Sigmoid), r=ZT[b], w=["m1"])
                b = zmm(wgb_, tgb_, half)
                P.op('act', lambda e, b=b: e.activation(m2[:], zp[b][:], AF.Sigmoid), r=ZT[b], w=["m2"])
                for tg in range(2):
                    t0 = half * 1024 + tg * 512
                    for kc in range(8):
                        P.op('pe', lambda e, tg=tg, kc=kc, t0=t0: e.matmul(gp[tg][:], wa_[:, kc, :], oaT[:, kc, t0:t0 + 512],
                                                                        start=(kc == 0), stop=(kc == 7)), r=[ta_, "oaT"], w=[GT[tg]])
                    for kc in range(8):
                        P.op('pe', lambda e, tg=tg, kc=kc, t0=t0: e.matmul(gp[2 + tg][:], wb_[:, kc, :], cvT[:, kc, t0:t0 + 512],
                                                                        start=(kc == 0), stop=(kc == 7)), r=[tb_] + CVT, w=[GT[2 + tg]])
                    P.op('dve', lambda e, tg=tg: e.tensor_tensor(m1[:, tg * 512:(tg + 1) * 512], gp[tg][:], m1[:, tg * 512:(tg + 1) * 512], ALU.mult),
                         r=[GT[tg], "m1"], w=["m1"])
                    P.op('dve', lambda e, tg=tg: e.tensor_tensor(m2[:, tg * 512:(tg + 1) * 512], gp[2 + tg][:], m2[:, tg * 512:(tg + 1) * 512], ALU.mult),
                         r=[GT[2 + tg], "m2"], w=["m2"])
                P.op('dve', lambda e, j=j, half=half: e.tensor_tensor(mergedT[:, j, half * 1024:(half + 1) * 1024], m1[:], m2[:], ALU.add),
                     r=["m1", "m2"], w=["mergedT"])
        phG.close()
        P.barrier(compute_only=True)

        def rowform(dst_ap, vv, stack):
            dgl = [sb(f"dg{vv}_{i}", [128, 128], F32, stack) for i in range(2)]
            for j in range(8):
                b_ = j % 2
                P.op('dve', lambda e, b_=b_, j=j: e.tensor_scalar(dgl[b_][:], ident[:], cvec[:, vv, j:j + 1], None, ALU.mult),
                     r=["ident", "cvec"], w=[f"dgl{vv}_{b_}"])
                pb = 2 + b_
                P.op('pe', lambda e, b_=b_, pb=pb: e.matmul(gp[pb][:, 0:128], ones_f[:], dgl[b_][:], start=True, stop=True),
                     r=["ones_f", f"dgl{vv}_{b_}"], w=[GT[pb]])
                P.op('act', lambda e, pb=pb, j=j: e.copy(dst_ap[:, j * 128:(j + 1) * 128], gp[pb][:, 0:128]), r=[GT[pb]], w=["rowv"])

        phY = ExitStack()
        rg1 = sb("rg1", [128, 1024], F32, phY)
        rowform(rg1, V_G1, phY)
        wout = sb("wout", [128, 8, 1024], BF16, phY)
        wov = w_out.rearrange("(kc p) n -> p kc n", p=128)
        for kc in range(8):
            P.dma('pool', lambda e, kc=kc: e.dma_start(out=wout[:, kc, :], in_=wov[:, kc, :]), w=["wout"])
        xta = [sb(f"xta{i}", [128, 1024], F32, phY) for i in range(2)]
        h1a = [sb(f"h1a{i}", [128, 1024], F32, phY) for i in range(2)]
        for i in range(NT):
            b = i % 2
            P.dma('sp', lambda e, i=i, b=b: e.dma_start(out=xta[b][:], in_=x[i * 128:(i + 1) * 128, :]), w=[f"xta{b}"])
            for ng in range(2):
                for kc in range(8):
                    P.op('pe', lambda e, i=i, b=b, ng=ng, kc=kc: e.matmul(zp[b][:, ng * 512:(ng + 1) * 512], mergedT[:, kc, i * 128:(i + 1) * 128],
                                                                        wout[:, kc, ng * 512:(ng + 1) * 512], start=(kc == 0), stop=(kc == 7)),
                         r=["mergedT", "wout"], w=[ZT[b][ng]])
            P.op('dve', lambda e, b=b: e.tensor_tensor(h1a[b][:], zp[b][:], rg1[:], ALU.mult), r=ZT[b] + ["rowv"], w=[f"h1a{b}"])
            P.op('dve', lambda e, b=b: e.tensor_tensor(h1a[b][:], h1a[b][:], xta[b][:], ALU.add), r=[f"h1a{b}", f"xta{b}"], w=[f"h1a{b}"])
            P.dma('sp', lambda e, i=i, b=b: e.dma_start(out=h1s[i * 128:(i + 1) * 128, :], in_=h1a[b][:]), r=[f"h1a{b}"], w=["h1s"])
        phY.close()
        phM.close()
        P.barrier()

        rowv = sb("rowv", [128, 2, 1024], F32)
        rowform(rowv[:, 0, :], V_G2, ES)
        rowform(rowv[:, 1, :], V_FG, ES)
        RG2, RFG = rowv[:, 0, :], rowv[:, 1, :]
        wqb = sb("wqb", [128, 8, 2048], BF16)
        keysT = sb("keysT", [128, 16, 128], BF16)
        s_ = sb("s_", [128, 16, 128], F32)
        wst = s_[:].rearrange("p a b -> p (a b)")
        wqv = peer_wq.rearrange("(kc p) n -> p kc n", p=128)
        for kc in range(8):
            P.dma('pool', lambda e, kc=kc: e.dma_start(out=wqb[:, kc, :], in_=wqv[:, kc, :]), w=["wqb"])
        kv_ = peer_keys.rearrange("g n d -> n g d")
        P.dma('sp', lambda e: e.dma_start(out=s_[:], in_=kv_), w=["s_"])
        for g in range(16):
            pb = g % 4
            P.op('pe', lambda e, g=g, pb=pb: e.transpose(gp[pb][:, 0:128], wst[:, g * 128:(g + 1) * 128], ident[:]),
                 r=["s_", "ident"], w=[GT[pb]])
            P.op('act', lambda e, g=g, pb=pb: e.copy(keysT[:, g, :], gp[pb][:, 0:128]), r=[GT[pb]], w=["keysT"])

        h1 = [sb(f"h1_{i}", [128, 1024], F32) for i in range(2)]
        u2 = [sb(f"u2_{i}", [128, 1024], BF16) for i in range(2)]
        prodb = [sb(f"prodb{i}", [128, 1024], BF16) for i in range(2)]
        xn2 = sb("xn2", [128, 1024], F32)
        jk = sb("jk", [128, 1024], BF16)
        fin = sb("fin", [128, 1024], F32)
        u2T = sb("u2T", [128, 8, 128], BF16)
        qT = sb("qT", [128, 16, 128], BF16)
        s2_ = sb("s2_", [128, 16, 128], F32)
        sc_ = sb("sc_", [128, 16, 16], F32)
        idx_ = sb("idx_", [128, 16, 16], U32)
        idxf = sb("idxf", [128, 16, 16], F32)
        cand = s_[:].rearrange("p a b -> p (a b)").rearrange("p (h c) -> p h c", c=256)
        cand2 = s2_[:].rearrange("p a b -> p (a b)").rearrange("p (h c) -> p h c", c=256)
        oh = s2_[:].rearrange("p a b -> p (a b)").rearrange("p (h k a) -> p h k a", k=16, a=16)
        top_ = sb("top_", [128, 8, 16], F32)
        pos_ = sb("pos_", [128, 8, 16], U32)
        pa_ = sb("pa_", [128, 8, 16], U32)
        paf = sb("paf", [128, 8, 16], F32)
        pbf = sb("pbf", [128, 8, 16], F32)
        isel = sb("isel", [128, 128], F32)
        jsel = sb("jsel", [128, 128], F32)
        eidx = [sb(f"eidx{i}", [128, 128], U32) for i in range(2)]
        gate = [sb(f"gate{i}", [128, 128], F32) for i in range(2)]
        gsum = sb("gsum", [128, 8], F32)
        act_ = sb("act_", [128, 128], F32)
        gl_ = sb("gl_", [128, 128], F32)
        st2 = sb("st2", [128, 8], F32)
        st3 = sb("st3", [128, 8], F32)
        io16 = sb("io16", [128, 16], F32)
        NGB = 22
        GB = [sb(f"GB{i}", [128, 2048], BF16) for i in range(NGB)]
        dgs = [sb(f"dgs{i}", [128, 128], BF16) for i in range(4)]
        P.op('dve', lambda e: e.tensor_copy(io16[:], iot_i[:, 0:16]), r=["iot_i"], w=["io16"])
        NEG = -1.0e30

        def front(i):
            b = i % 2
            H1, U2, EIDX, GATE = h1[b], u2[b], eidx[b], gate[b]
            g3 = GATE[:].rearrange("p (h k) -> p h k", k=16)
            P.dma('sp', lambda e: e.dma_start(out=H1[:], in_=h1s[i * 128:(i + 1) * 128, :]), r=["h1s"], w=[f"h1_{b}"])
            yield
            P.op('dve', lambda e: e.scalar_tensor_tensor(xn2[:], H1[:], 1.0, H1[:], ALU.mult, ALU.mult, accum_out=st2[:, 0:1]),
                 r=[f"h1_{b}"], w=["xn2", "st2"])
            yield
            P.op('act', lambda e: e.activation(st2[:, 1:2], st2[:, 0:1], AF.Sqrt, scale=1.0 / D, bias=EPS), r=["st2"], w=["st2"])
            P.op('dve', lambda e: e.reciprocal(st2[:, 2:3], st2[:, 1:2]), r=["st2"], w=["st2"])
            yield
            P.op('dve', lambda e: e.tensor_scalar(xn2[:], H1[:], st2[:, 2:3], None, ALU.mult), r=[f"h1_{b}", "st2"], w=["xn2"])
            yield
            yield
            for j in range(8):
                pb = j // 4
                P.op('pe', lambda e, j=j, pb=pb: e.transpose(gp[pb][:, (j % 4) * 128:(j % 4 + 1) * 128], xn2[:, j * 128:(j + 1) * 128], ident[:]),
                     r=["xn2", "ident"], w=[GT[pb]])
            for j in range(8):
                pb = j // 4
                P.op('act', lambda e, j=j, pb=pb: e.activation(u2T[:, j, :], gp[pb][:, (j % 4) * 128:(j % 4 + 1) * 128], AF.Identity,
                                                             scale=cvec[:, V_W2, j:j + 1], bias=cvec[:, V_SH2, j:j + 1]),
                     r=[GT[pb], "cvec"], w=["u2T"])
            gpb2 = gp[2][:].bitcast(BF16)
            for j in range(8):
                P.op('pe', lambda e, j=j: e.transpose(gpb2[:, j * 128:(j + 1) * 128], u2T[:, j, :], identb[:]), r=["u2T", "identb"], w=[GT[2]])
            P.op('act', lambda e: e.copy(U2[:], gpb2), r=[GT[2]], w=[f"u2_{b}"])
            yield
            for g in range(16):
                pb = 2 + (g // 4) % 2
                for kc in range(8):
                    P.op('pe', lambda e, g=g, kc=kc, pb=pb: e.matmul(gp[pb][:, (g % 4) * 128:(g % 4 + 1) * 128], wqb[:, kc, g * 128:(g + 1) * 128],
                                                                   u2T[:, kc, :], start=(kc == 0), stop=(kc == 7)), r=["wqb", "u2T"], w=[GT[pb]])
                if g % 4 == 3:
                    P.op('act', lambda e, g=g, pb=pb: e.copy(qT[:, g - 3:g + 1, :].rearrange("p a b -> p (a b)"), gp[pb][:]), r=[GT[pb]], w=["qT"])
                    yield
            for g in range(16):
                pb = (g // 4) % 2
                P.op('pe', lambda e, g=g, pb=pb: e.matmul(gp[pb][:, (g % 4) * 128:(g % 4 + 1) * 128], qT[:, g, :], keysT[:, g, :],
                                                        start=True, stop=True), r=["qT", "keysT"], w=[GT[pb]])
                if g % 4 == 3:
                    P.op('act', lambda e, g=g, pb=pb: e.copy(s_[:, g - 3:g + 1, :].rearrange("p a b -> p (a b)"), gp[pb][:]), r=[GT[pb]], w=["s_"])
            yield
            for g in range(16):
                P.op('dve', lambda e, g=g: e.max(sc_[:, g, 0:8], s_[:, g, :]), r=["s_"], w=["sc_"])
                yield
                P.op('dve', lambda e, g=g: e.match_replace(s2_[:, g, :], sc_[:, g, 0:8], s_[:, g, :], NEG), r=["s_", "sc_"], w=["s2_"])
                yield
                P.op('dve', lambda e, g=g: e.max(sc_[:, g, 8:16], s2_[:, g, :]), r=["s2_"], w=["sc_"])
                yield
                P.op('dve', lambda e, g=g: e.max_index(idx_[:, g, 0:8], sc_[:, g, 0:8], s_[:, g, :]), r=["s_", "sc_"], w=["idx_"])
                yield
                P.op('dve', lambda e, g=g: e.max_index(idx_[:, g, 8:16], sc_[:, g, 8:16], s_[:, g, :]), r=["s_", "sc_"], w=["idx_"])
                yield
                if g % 2 == 1:
                    yield
            P.op('dve', lambda e: e.tensor_copy(idxf[:], idx_[:]), r=["idx_"], w=["idxf"])
            yield
            sc4 = sc_[:].rearrange("p (h two) k -> p h two k", two=2)
            ix4 = idxf[:].rearrange("p (h two) k -> p h two k", two=2)
            P.op('dve', lambda e: e.tensor_tensor(cand.rearrange("p h (a b) -> p h a b", b=16),
                                                  sc4[:, :, 0, :].unsqueeze(3).to_broadcast([128, 8, 16, 16]),
                                                  sc4[:, :, 1, :].unsqueeze(2).to_broadcast([128, 8, 16, 16]), ALU.add),
                 r=["sc_"], w=["s_"])
            yield
            yield
            for hh in range(8):
                P.op('dve', lambda e, hh=hh: e.max(top_[:, hh, 0:8], cand[:, hh, :]), r=["s_"], w=["top_"])
                yield
                P.op('dve', lambda e, hh=hh: e.match_replace(cand2[:, hh, :], top_[:, hh, 0:8], cand[:, hh, :], NEG), r=["s_", "top_"], w=["s2_"])
                yield
                P.op('dve', lambda e, hh=hh: e.max(top_[:, hh, 8:16], cand2[:, hh, :]), r=["s2_"], w=["top_"])
                yield
                P.op('dve', lambda e, hh=hh: e.max_index(pos_[:, hh, 0:8], top_[:, hh, 0:8], cand[:, hh, :]), r=["s_", "top_"], w=["pos_"])
                yield
                P.op('dve', lambda e, hh=hh: e.max_index(pos_[:, hh, 8:16], top_[:, hh, 8:16], cand[:, hh, :]), r=["s_", "top_"], w=["pos_"])
                yield
                yield
            P.op('dve', lambda e: e.tensor_scalar(pa_[:], pos_[:], 4, None, ALU.logical_shift_right), r=["pos_"], w=["pa_"])
            yield
            P.op('dve', lambda e: e.tensor_copy(paf[:], pa_[:]), r=["pa_"], w=["paf"])
            yield
            P.op('dve', lambda e: e.tensor_scalar(pa_[:], pos_[:], 15, None, ALU.bitwise_and), r=["pos_"], w=["pa_"])
            yield
            P.op('dve', lambda e: e.tensor_copy(pbf[:], pa_[:]), r=["pa_"], w=["pbf"])
            yield
            yield
            for which, (pf, dst) in enumerate(((paf, isel), (pbf, jsel))):
                P.op('dve', lambda e, pf=pf: e.tensor_tensor(oh, pf[:].unsqueeze(3).to_broadcast([128, 8, 16, 16]),
                                                             io16[:].unsqueeze(1).unsqueeze(1).to_broadcast([128, 8, 16, 16]), ALU.is_equal),
                     r=["paf", "pbf", "io16"], w=["s2_"])
                yield
                P.op('dve', lambda e, which=which: e.tensor_tensor(oh, oh, ix4[:, :, which, :].unsqueeze(2).to_broadcast([128, 8, 16, 16]), ALU.mult),
                     r=["s2_", "idxf"], w=["s2_"])
                yield
                P.op('dve', lambda e, dst=dst: e.tensor_reduce(dst[:], oh.rearrange("p h k a -> p (h k) a"), AX.X, ALU.add),
                     r=["s2_"], w=["isel", "jsel"])
                yield
                yield
            P.op('dve', lambda e: e.scalar_tensor_tensor(isel[:], isel[:], 128.0, jsel[:], ALU.mult, ALU.add), r=["isel", "jsel"], w=["isel"])
            yield
            P.op('dve', lambda e: e.tensor_copy(EIDX[:], isel[:]), r=["isel"], w=[f"eidx{b}"])
            yield
            P.op('dve', lambda e: e.tensor_tensor(g3, top_[:], top_[:, :, 0:1].to_broadcast([128, 8, 16]), ALU.subtract), r=["top_"], w=[f"gate{b}"])
            yield
            P.op('act', lambda e: e.activation(GATE[:], GATE[:], AF.Exp), r=[f"gate{b}"], w=[f"gate{b}"])
            P.op('dve', lambda e: e.tensor_reduce(gsum[:], g3, AX.X, ALU.add), r=[f"gate{b}"], w=["gsum"])
            yield
            P.op('dve', lambda e: e.reciprocal(gsum[:], gsum[:]), r=["gsum"], w=["gsum"])
            yield
            P.op('dve', lambda e: e.tensor_tensor(g3, g3, gsum[:].unsqueeze(2).to_broadcast([128, 8, 16]), ALU.mult), r=[f"gate{b}", "gsum"], w=[f"gate{b}"])
            yield
            yield

        def back(i, nxt):
            b = i % 2
            H1, U2, EIDX, GATE = h1[b], u2[b], eidx[b], gate[b]
            G4 = 4

            def slot_front(sl):
                k = sl % NGB
                P.dma('pool', lambda e: e.indirect_dma_start(out=GB[k][:], out_offset=None, in_=tabUV,
                                                             in_offset=bass.IndirectOffsetOnAxis(ap=EIDX[:, sl:sl + 1], axis=0)),
                      r=[f"eidx{b}"], w=[f"GB{k}"])
                if sl % 2 == 0:
                    P.op('dve', lambda e: e.scalar_tensor_tensor(jk[:], GB[k][:, 0:1024], 1.0, U2[:], ALU.mult, ALU.mult,
                                                                 accum_out=act_[:, sl:sl + 1]),
                         r=[f"GB{k}", f"u2_{b}"], w=["jk", f"act{sl}"])
                else:
                    pq = (sl // 2) % 2
                    P.op('dve', lambda e: e.tensor_tensor(prodb[pq][:], GB[k][:, 0:1024], U2[:], ALU.mult),
                         r=[f"GB{k}", f"u2_{b}"], w=[f"prodb{pq}"])
                    P.op('act', lambda e: e.activation(prodb[pq][:], prodb[pq][:], AF.Copy, accum_out=act_[:, sl:sl + 1]),
                         r=[f"prodb{pq}"], w=[f"prodb{pq}", f"act{sl}"])

            NDG = 4

            def st_gelu(sl):
                P.op('act', lambda e: e.activation(gl_[:, sl:sl + 1], act_[:, sl:sl + 1], AF.Gelu), r=[f"act{sl}"], w=[f"gl{sl}"])

            def st_gate(sl):
                P.op('dve', lambda e: e.tensor_tensor(gl_[:, sl:sl + 1], gl_[:, sl:sl + 1], GATE[:, sl:sl + 1], ALU.mult),
                     r=[f"gl{sl}", f"gate{b}"], w=[f"gl{sl}"])

            def st_diag(sl):
                k2 = sl % NDG
                P.op('act', lambda e: e.activation(dgs[k2][:], ident[:], AF.Copy, scale=gl_[:, sl:sl + 1]),
                     r=["ident", f"gl{sl}"], w=[f"dgs{k2}"])

            def st_mm(sl):
                k = sl % NGB
                k2 = sl % NDG
                for ng in range(2):
                    P.op('pe', lambda e, ng=ng: e.matmul(zp[1][:, ng * 512:(ng + 1) * 512], dgs[k2][:],
                                                       GB[k][:, 1024 + ng * 512:1024 + (ng + 1) * 512],
                                                       start=(sl == 0), stop=(sl == 127)),
                         r=[f"dgs{k2}", f"GB{k}"], w=[ZT[1][ng]])

            L1, L2, L3, L4 = 2, 4, 6, 8
            for p_ in range(128 + L4):
                if p_ < 128:
                    slot_front(p_)
                    if nxt is not None:
                        next(nxt, None)
                        if p_ % 2 == 1:
                            next(nxt, None)
                for L, fn in ((L1, st_gelu), (L2, st_gate), (L3, st_diag), (L4, st_mm)):
                    if 0 <= p_ - L < 128:
                        fn(p_ - L)
            if nxt is not None:
                for _ in nxt:
                    pass
            P.op('dve', lambda e: e.tensor_tensor(fin[:], zp[1][:], RG2, ALU.mult), r=ZT[1] + ["rowv"], w=["fin"])
            P.op('dve', lambda e: e.tensor_tensor(H1[:], H1[:], fin[:], ALU.add), r=[f"h1_{b}", "fin"], w=[f"h1_{b}"])
            P.op('dve', lambda e: e.scalar_tensor_tensor(fin[:], H1[:], 1.0, H1[:], ALU.mult, ALU.mult, accum_out=st3[:, 0:1]),
                 r=[f"h1_{b}"], w=["fin", "st3"])
            P.op('act', lambda e: e.activation(st3[:, 1:2], st3[:, 0:1], AF.Sqrt, scale=1.0 / D, bias=EPS), r=["st3"], w=["st3"])
            P.op('dve', lambda e: e.reciprocal(st3[:, 2:3], st3[:, 1:2]), r=["st3"], w=["st3"])
            P.op('dve', lambda e: e.scalar_tensor_tensor(fin[:], H1[:], st3[:, 2:3], RFG, ALU.mult, ALU.mult), r=[f"h1_{b}", "st3", "rowv"], w=["fin"])
            P.dma('sp', lambda e: e.dma_start(out=out[i * 128:(i + 1) * 128, :], in_=fin[:]), r=["fin"], w=["out"])

        for _ in front(0):
            pass
        for i in range(NT):
            back(i, front(i + 1) if i + 1 < NT else None)

        P.finish(["out"])
    return nc


_CACHE = {}


def kernel(**inputs):
    inp = {k: np.ascontiguousarray(np.asarray(v, dtype=np.float32)) for k, v in inputs.items()}
    if "nc" not in _CACHE:
        _CACHE["nc"] = build_nc()
    nc = _CACHE["nc"]
    shared = {
        "ada_w": inp["ada_w"][0], "ada_b": inp["ada_b"][0], "norm1_g": inp["norm1_g"][0], "w_in": inp["w_in"][0],
        "lb_logits": inp["lb_logits"], "hg_norm_g": inp["hg_norm_g"][0], "w_a": inp["w_a"][0],
        "conv_w": inp["conv_w"][0], "conv_b": inp["conv_b"][0], "conv_ln_g": inp["conv_ln_g"][0],
        "conv_ln_b": inp["conv_ln_b"][0], "w_b": inp["w_b"][0], "w_out": inp["w_out"][0],
        "norm2_g": inp["norm2_g"][0], "peer_wq": inp["peer_wq"][0],
        "peer_keys": inp["peer_keys"][0].reshape(16, 128, 128), "peer_u": inp["peer_u"][0],
        "peer_v": inp["peer_v"][0], "final_g": inp["final_g"],
    }
    in_maps = []
    for b in range(NCORES):
        m = dict(shared)
        m["x"] = inp["x"][b]
        m["c"] = inp["c"][b]
        in_maps.append(m)
    res = run_bass_kernel_spmd(nc, in_maps, core_ids=list(range(NCORES)))
    return np.stack([np.asarray(r["out"], dtype=np.float32) for r in res.results], axis=0)
```
